# Optimizing a Trainium2 kernel written in Bass

```python
import math
import jax, jax.numpy as jnp
from jax import lax
import numpy as np

D_MODEL = 1024
BATCH = 4
SEQ = 8192
DEPTH = 4

CHUNK = 64
Q_BLOCK = 128
N_MEM = 256
EPS = 1e-6
NEG_INF = -1e30

H_A = 8
DH_A = 64
D_LAT = 128
H_IDX = 8
DH_IDX = 64
TOPK_MAX = 256

H_B = 4
DH_B = 64

H_C = 16
H_C_KV = 2
G_C = H_C // H_C_KV
DH_C = 64
WINDOW = 128
WIN_CHUNKS = WINDOW // CHUNK
BAND = (WIN_CHUNKS + 1) * CHUNK

H_X = 4
DH_X = 64

D_FF = 4 * D_MODEL

N_BUCKETS = 32
MAX_DIST = 1024
H_BIAS = H_A + H_B + H_C

N_EVEN = (DEPTH + 1) // 2
N_ODD = DEPTH // 2

EVEN_SIZES = (H_A * DH_A, D_LAT, H_IDX * DH_IDX, DH_IDX, H_IDX, 2 * H_B * DH_B, 2 * H_B * DH_B, 2 * H_B * DH_B)
EVEN_COLS = sum(EVEN_SIZES)
ODD_SIZES = (H_C * DH_C, H_C_KV * DH_C, H_C_KV * DH_C)
ODD_COLS = sum(ODD_SIZES)

kernel_name = "hybrid_dsa_diff_swa_streaming_block"


def _split_points(sizes):
    pts, acc = [], 0
    for s in sizes[:-1]:
        acc += s
        pts.append(acc)
    return pts


def rms_norm(x, g):
    xf = x.astype(jnp.float32)
    y = xf * lax.rsqrt(jnp.mean(xf * xf, axis=-1, keepdims=True) + EPS)
    return (y * g.astype(jnp.float32)).astype(x.dtype)


def t5_bucket(rel):
    nb = N_BUCKETS // 2
    max_exact = nb // 2
    n = jnp.abs(rel)
    nf = jnp.maximum(n, 1).astype(jnp.float32)
    large = max_exact + (jnp.log(nf / max_exact) / math.log(MAX_DIST / max_exact) * (nb - max_exact)).astype(jnp.int32)
    large = jnp.minimum(large, nb - 1)
    return jnp.where(rel > 0, nb, 0) + jnp.where(n < max_exact, n, large)


def chunk_id(pos):
    return pos // CHUNK


def dsa_attention(q_a, c_kv, q_idx, k_idx, w_idx, w_uk, w_uv, table_a):
    B, S = q_a.shape[0], q_a.shape[1]
    topk = min(TOPK_MAX, S // 4)
    n_blk = S // Q_BLOCK
    spos = jnp.arange(S)
    scale = DH_A ** -0.5
    q_lat = jnp.einsum('bshd,hdl->bshl', q_a, w_uk)

    def block(i):
        t0 = i * Q_BLOCK
        tpos = t0 + jnp.arange(Q_BLOCK)
        ql = lax.dynamic_slice_in_dim(q_lat, t0, Q_BLOCK, axis=1)
        qi = lax.dynamic_slice_in_dim(q_idx, t0, Q_BLOCK, axis=1)
        wi = lax.dynamic_slice_in_dim(w_idx, t0, Q_BLOCK, axis=1)
        act = jax.nn.relu(jnp.einsum('bqhd,bsd->bqhs', qi, k_idx).astype(jnp.float32))
        score = jnp.einsum('bqhs,bqh->bqs', act, wi.astype(jnp.float32))
        adm = chunk_id(spos)[None, :] <= chunk_id(tpos)[:, None]
        score = jnp.where(adm[None], score, -jnp.inf)
        _, sel = lax.top_k(score, topk)
        valid = chunk_id(sel) <= chunk_id(tpos)[None, :, None]
        c_sel = jax.vmap(lambda c, ix: c[ix])(c_kv, sel)
        bias = table_a[t5_bucket(sel - tpos[None, :, None])]
        s = jnp.einsum('bqhl,bqkl->bqkh', ql, c_sel).astype(jnp.float32) * scale + bias
        s = jnp.where(valid[..., None], s, NEG_INF)
        p = jax.nn.softmax(s, axis=2)
        return jnp.einsum('bqkh,bqkl->bqhl', p.astype(c_sel.dtype), c_sel)

    o = lax.map(block, jnp.arange(n_blk))
    o = jnp.moveaxis(o, 0, 1).reshape(B, S, H_A, D_LAT)
    o = jnp.einsum('bshl,hld->bshd', o, w_uv)
    return o.reshape(B, S, H_A * DH_A)


def diff_attention(q_b, k_b, v_b, b_lambda, b_subln, table_b, lam_init):
    B, S = q_b.shape[0], q_b.shape[1]
    n_blk = S // Q_BLOCK
    spos = jnp.arange(S)
    scale = DH_B ** -0.5
    lf = b_lambda.astype(jnp.float32)
    lam = jnp.exp(jnp.sum(lf[0] * lf[1])) - jnp.exp(jnp.sum(lf[2] * lf[3])) + lam_init

    def block(i):
        t0 = i * Q_BLOCK
        tpos = t0 + jnp.arange(Q_BLOCK)
        qb = lax.dynamic_slice_in_dim(q_b, t0, Q_BLOCK, axis=1)
        bias = jnp.moveaxis(table_b[t5_bucket(spos[None, :] - tpos[:, None])], -1, 0)
        adm = chunk_id(spos)[None, :] <= chunk_id(tpos)[:, None]
        s = jnp.einsum('bqhmd,bshmd->bhmqs', qb, k_b).astype(jnp.float32) * scale + bias[None, :, None]
        s = jnp.where(adm, s, NEG_INF)
        p = jax.nn.softmax(s, axis=-1)
        a = p[:, :, 0] - lam * p[:, :, 1]
        return jnp.einsum('bhqs,bshe->bqhe', a.astype(v_b.dtype), v_b)

    o = lax.map(block, jnp.arange(n_blk))
    o = jnp.moveaxis(o, 0, 1).reshape(B, S, H_B, 2 * DH_B)
    o = rms_norm(o, b_subln) * (1.0 - lam_init)
    return o.reshape(B, S, H_B * 2 * DH_B)


def even_mixer(h, w_in, a_kv_norm, w_uk, w_uv, b_lambda, b_subln, w_out, table, lam_init):
    B, S, _ = h.shape
    z = h @ w_in
    q_a, c_kv, q_i, k_i, w_i, q_b, k_b, v_b = jnp.split(z, _split_points(EVEN_SIZES), axis=-1)
    c_kv = rms_norm(c_kv, a_kv_norm)
    o_a = dsa_attention(q_a.reshape(B, S, H_A, DH_A), c_kv, q_i.reshape(B, S, H_IDX, DH_IDX), k_i, w_i,
                        w_uk, w_uv, table[:, :H_A])
    o_b = diff_attention(q_b.reshape(B, S, H_B, 2, DH_B), k_b.reshape(B, S, H_B, 2, DH_B),
                         v_b.reshape(B, S, H_B, 2 * DH_B), b_lambda, b_subln, table[:, H_A:H_A + H_B], lam_init)
    return jnp.concatenate([o_a, o_b], axis=-1) @ w_out


def odd_mixer(h, w_in, sinks, w_out, table):
    B, S, _ = h.shape
    nc = S // CHUNK
    q, k, v = jnp.split(h @ w_in, _split_points(ODD_SIZES), axis=-1)
    q = q.reshape(B, nc, CHUNK, H_C_KV, G_C, DH_C)
    k = k.reshape(B, nc, CHUNK, H_C_KV, DH_C)
    v = v.reshape(B, nc, CHUNK, H_C_KV, DH_C)

    def band(t):
        tp = jnp.pad(t, ((0, 0), (WIN_CHUNKS, 0), (0, 0), (0, 0), (0, 0)))
        return jnp.concatenate([tp[:, j:j + nc] for j in range(WIN_CHUNKS + 1)], axis=2)

    kb, vb = band(k), band(v)
    rel = (jnp.arange(BAND)[None, :] - WIN_CHUNKS * CHUNK) - jnp.arange(CHUNK)[:, None]
    bias = jnp.moveaxis(table[:, H_A + H_B:][t5_bucket(rel)], -1, 0).reshape(H_C_KV, G_C, CHUNK, BAND)
    s = jnp.einsum('bcqkgd,bcskd->bckgqs', q, kb).astype(jnp.float32) * (DH_C ** -0.5) + bias
    valid = (jnp.arange(nc)[:, None] - WIN_CHUNKS + jnp.arange(BAND)[None, :] // CHUNK) >= 0
    s = jnp.where(valid[None, :, None, None, None, :], s, NEG_INF)
    sink = jnp.broadcast_to(sinks.astype(jnp.float32).reshape(H_C_KV, G_C)[None, None, :, :, None, None],
                            s.shape[:-1] + (1,))
    p = jax.nn.softmax(jnp.concatenate([s, sink], axis=-1), axis=-1)[..., :-1]
    o = jnp.einsum('bckgqs,bcskd->bcqkgd', p.astype(vb.dtype), vb).reshape(B, S, H_C * DH_C)
    return o @ w_out


def memory_cross_attention(h, mem, wq, wkv, wo, mem_norm):
    B, S, _ = h.shape
    M = mem.shape[1]
    q = (h @ wq).reshape(B, S, H_X, DH_X)
    kv = (rms_norm(mem, mem_norm) @ wkv).reshape(B, M, 2, H_X, DH_X)
    s = jnp.einsum('bshd,bmhd->bhsm', q, kv[:, :, 0]).astype(jnp.float32) * (DH_X ** -0.5)
    p = jax.nn.softmax(s, axis=-1)
    o = jnp.einsum('bhsm,bmhd->bshd', p.astype(kv.dtype), kv[:, :, 1]).reshape(B, S, H_X * DH_X)
    return o @ wo


def sq_relu_mlp(h, w1, w2):
    return jnp.square(jax.nn.relu(h @ w1)) @ w2


def setup_inputs(seed: int = 0) -> dict:
    key = jax.random.key(seed)
    ks = jax.random.split(key, 20)

    def nrm(k, shape, scale):
        return jax.random.normal(k, shape, jnp.float32) * scale

    D = D_MODEL
    return {
        "x": nrm(ks[0], (BATCH, SEQ, D), 1.0),
        "mem": nrm(ks[1], (BATCH, N_MEM, D), 1.0),
        "rel_bias_table": nrm(ks[2], (N_BUCKETS, H_BIAS), 0.3),
        "norm_g": 1.0 + nrm(ks[3], (DEPTH, 6, D), 0.05),
        "ev_w_in": nrm(ks[4], (N_EVEN, D, EVEN_COLS), D ** -0.5),
        "ev_a_kv_norm": 1.0 + nrm(ks[5], (N_EVEN, D_LAT), 0.05),
        "ev_a_w_uk": nrm(ks[6], (N_EVEN, H_A, DH_A, D_LAT), DH_A ** -0.5),
        "ev_a_w_uv": nrm(ks[7], (N_EVEN, H_A, D_LAT, DH_A), D_LAT ** -0.5),
        "ev_b_lambda": nrm(ks[8], (N_EVEN, 4, DH_B), 0.1),
        "ev_b_subln": 1.0 + nrm(ks[9], (N_EVEN, 2 * DH_B), 0.05),
        "ev_w_out": nrm(ks[10], (N_EVEN, H_A * DH_A + 2 * H_B * DH_B, D), D ** -0.5),
        "od_w_in": nrm(ks[11], (N_ODD, D, ODD_COLS), D ** -0.5),
        "od_sinks": nrm(ks[12], (N_ODD, H_C), 0.5),
        "od_w_out": nrm(ks[13], (N_ODD, H_C * DH_C, D), (H_C * DH_C) ** -0.5),
        "xa_wq": nrm(ks[14], (DEPTH, D, H_X * DH_X), D ** -0.5),
        "xa_wkv": nrm(ks[15], (DEPTH, D, 2 * H_X * DH_X), D ** -0.5),
        "xa_wo": nrm(ks[16], (DEPTH, H_X * DH_X, D), (H_X * DH_X) ** -0.5),
        "xa_mem_norm": 1.0 + nrm(ks[17], (DEPTH, D), 0.05),
        "mlp_w1": nrm(ks[18], (DEPTH, D, D_FF), D ** -0.5),
        "mlp_w2": nrm(ks[19], (DEPTH, D_FF, D), D_FF ** -0.5),
    }


def reference(x, mem, rel_bias_table, norm_g, ev_w_in, ev_a_kv_norm, ev_a_w_uk, ev_a_w_uv, ev_b_lambda,
              ev_b_subln, ev_w_out, od_w_in, od_sinks, od_w_out, xa_wq, xa_wkv, xa_wo, xa_mem_norm,
              mlp_w1, mlp_w2):
    for l in range(DEPTH):
        g = norm_g[l]
        h = rms_norm(x, g[0])
        if l % 2 == 0:
            e = l // 2
            lam_init = 0.8 - 0.6 * math.exp(-0.3 * l)
            y = even_mixer(h, ev_w_in[e], ev_a_kv_norm[e], ev_a_w_uk[e], ev_a_w_uv[e], ev_b_lambda[e],
                           ev_b_subln[e], ev_w_out[e], rel_bias_table, lam_init)
        else:
            o = l // 2
            y = odd_mixer(h, od_w_in[o], od_sinks[o], od_w_out[o], rel_bias_table)
        x = x + rms_norm(y, g[1])
        h = rms_norm(x, g[2])
        y = memory_cross_attention(h, mem, xa_wq[l], xa_wkv[l], xa_wo[l], xa_mem_norm[l])
        x = x + rms_norm(y, g[3])
        h = rms_norm(x, g[4])
        y = sq_relu_mlp(h, mlp_w1[l], mlp_w2[l])
        x = x + rms_norm(y, g[5])
    return x
```

```python
import math
from contextlib import ExitStack
import numpy as np
import concourse.bass as bass
import concourse.mybir as mybir
from concourse.bass_utils import run_bass_kernel_spmd

F32 = mybir.dt.float32
BF16 = mybir.dt.bfloat16
AF = mybir.ActivationFunctionType
ALU = mybir.AluOpType
AX = mybir.AxisListType

T = 8192
NT = 64
NG = 16
D = 1024
EPS = 1e-6
NEG = -30000.0
DEPTH = 4
EV_COLS = 2760
OD_COLS = 1280
SAME_SYNC = True
NBIS = 16


class Sem:
    def __init__(self, h):
        self.h = h
        self.count = 0


class Buf:
    def __init__(self, name, t=None):
        self.name = name
        self.t = t
        self.w = None
        self.rs = {}
        self.dsem = None
        self.excl = False


class Eng:
    def __init__(self, name, obj, sem):
        self.name = name
        self.obj = obj
        self.sem = sem
        self.known = {}
        self.issued = {}


class Stage:
    def __init__(self, k):
        self.k = k
        self.es = ExitStack()
        self.bufs = []

    def sb(self, name, shape, dt):
        self.k.uid += 1
        t = self.es.enter_context(self.k.nc.sbuf_tensor(f"{name}_{self.k.uid}", shape, dt))
        b = Buf(name, t)
        self.bufs.append(b)
        return b

    def __enter__(self):
        return self

    def __exit__(self, *a):
        self.k.barrier()
        for b in self.bufs:
            if b.dsem is not None:
                for qn_, s_ in b.dsem.items():
                    self.k.free_dsems[qn_].append(s_)
                b.dsem = None
        self.es.close()
        return False


class K:
    def __init__(self, nc, es):
        self.nc = nc
        self.uid = 0
        self.E = {}
        for name in ("tensor", "vector", "scalar", "gpsimd", "sync"):
            s = Sem(es.enter_context(nc.semaphore(f"e_{name}")))
            self.E[name] = Eng(name, getattr(nc, name), s)
        self.free_dsems = {"sync": [], "gpsimd": []}
        for i in range(44):
            try:
                self.free_dsems["sync" if i % 2 == 0 else "gpsimd"].append(Sem(es.enter_context(nc.semaphore(f"d{i}"))))
            except KeyError:
                break
        self.pp = []
        self.P = []
        for i in range(4):
            t = es.enter_context(nc.psum_tensor(f"pp{i}", [128, 1024], F32))
            self.pp.append(t)
            self.P.append(Buf(f"P{2*i}"))
            self.P.append(Buf(f"P{2*i+1}"))
            self.P[-1].excl = True
            self.P[-2].excl = True
        self.rr = 0

    def pb(self, k):
        return self.pp[k // 2][:, (k % 2) * 512:(k % 2 + 1) * 512]

    def pb2(self, i):
        return self.pp[i][:, :]

    def pbb(self, k):
        return self.pp[k // 2][:, (k % 2) * 512:(k % 2 + 1) * 512].bitcast(BF16)

    def _deps(self, reads, writes):
        toks = {}
        writes = list(writes) + [b for b in reads if b.excl]

        def add(sem, v):
            if toks.get(sem, 0) < v:
                toks[sem] = v
        for b in reads:
            if b.w is not None:
                add(*b.w)
        for b in writes:
            if b.w is not None:
                add(*b.w)
            for sem, v in b.rs.items():
                add(sem, v)
        return toks

    def _wait(self, eng, toks, skip=None):
        for sem, v in toks.items():
            if sem is skip:
                continue
            if sem is eng.sem and (eng.name == "tensor" or not SAME_SYNC):
                continue
            if eng.known.get(sem, 0) >= v:
                continue
            eng.obj.wait_ge(sem.h, v)
            eng.known[sem] = v

    def _mark(self, tok, reads, writes):
        writes = list(writes) + [b for b in reads if b.excl]
        reads = [b for b in reads if not b.excl]
        for b in reads:
            if b.rs.get(tok[0], 0) < tok[1]:
                b.rs[tok[0]] = tok[1]
        for b in writes:
            b.w = tok
            b.rs = {}

    def op(self, engname, fn, reads=(), writes=()):
        eng = self.E[engname]
        self._wait(eng, self._deps(reads, writes))
        ins = fn(eng.obj)
        eng.sem.count += 1
        ins.then_inc(eng.sem.h, 1)
        self._mark((eng.sem, eng.sem.count), reads, writes)
        return ins

    def pe(self, fns, reads=(), writes=()):
        eng = self.E["tensor"]
        self._wait(eng, self._deps(reads, writes))
        ins = None
        for f in fns:
            ins = f(eng.obj)
        eng.sem.count += 1
        ins.then_inc(eng.sem.h, 1)
        self._mark((eng.sem, eng.sem.count), reads, writes)

    def dma(self, qname, out, in_, reads=(), writes=(), sembuf=None, **kw):
        q = self.E[qname]
        b = sembuf or (writes[0] if writes else reads[0])
        if b.dsem is None:
            b.dsem = {}
        if qname not in b.dsem:
            b.dsem[qname] = self.free_dsems[qname].pop()
        ds = b.dsem[qname]
        self._wait(q, self._deps(reads, writes), skip=ds)
        ins = q.obj.dma_start(out=out, in_=in_, **kw)
        ds.count += 16
        ins.then_inc(ds.h, 16)
        q.issued[ds] = ds.count
        self._mark((ds, ds.count), reads, writes)

    def barrier(self):
        for qn in ("sync", "gpsimd", "scalar", "vector"):
            q = self.E[qn]
            for sem, v in list(q.issued.items()):
                if q.known.get(sem, 0) < v:
                    q.obj.wait_ge(sem.h, v)
                    q.known[sem] = v
            q.issued = {}
        q = self.E["sync"]
        q.obj.sem_inc(q.sem.h, 1)
        q.sem.count += 1
        g = self.E["gpsimd"]
        g.obj.sem_inc(g.sem.h, 1)
        g.sem.count += 1
        for e in self.E.values():
            for o in self.E.values():
                if o is e:
                    continue
                if e.known.get(o.sem, 0) < o.sem.count:
                    e.obj.wait_ge(o.sem.h, o.sem.count)
                    e.known[o.sem] = o.sem.count
        for p in self.P:
            p.w = None
            p.rs = {}

    def evac_eng(self):
        self.rr += 1
        return "vector" if self.rr % 2 else "scalar"

    def copy(self, engname, out, in_, reads, writes):
        if engname == "scalar":
            return self.op("scalar", lambda e: e.copy(out=out, in_=in_), reads, writes)
        return self.op(engname, lambda e: e.tensor_copy(out=out, in_=in_), reads, writes)


def t5_bucket_np(rel):
    nb = 16
    max_exact = 8
    n = np.abs(rel)
    nf = np.maximum(n, 1).astype(np.float32)
    large = max_exact + (np.log(nf / max_exact) / math.log(1024 / max_exact) * (nb - max_exact)).astype(np.int32)
    large = np.minimum(large, nb - 1)
    return np.where(rel > 0, nb, 0) + np.where(n < max_exact, n, large)


def host_consts():
    s = np.arange(128)[:, None]
    q = np.arange(128)[None, :]
    oh = np.zeros((32, 6, 128, 128), np.float32)
    for d in range(6):
        rel = (s - q) - 128 * d
        b = t5_bucket_np(rel)
        for bb in range(32):
            oh[bb, d] = (b == bb)
    oh = oh.reshape(32, 6 * 16384)
    c128 = np.zeros((128, 16 + 128 + 128), np.float32)
    c128[:, 0:16] = (2.0 ** -np.arange(16))[None, :]
    qq = np.arange(128)[:, None]
    ss = np.arange(128)[None, :]
    c128[:, 16:144] = np.where((qq < 64) & (ss >= 64), NEG, 0.0)
    c128[:, 144:272] = np.eye(128, dtype=np.float32)
    hmask = np.zeros((28, 1), np.float32)
    hmask[:12] = 1.0
    return oh, c128, hmask


def build(stop=None, debug=(), lim=16):
    NGR = lim
    NTR = 4 * lim
    nc = bass.Bass("TRN2", target_bir_lowering=False)

    def din(name, shape, dt=F32):
        return nc.dram_tensor(name, list(shape), dt, kind="ExternalInput").ap()

    def dtmp(name, shape, dt):
        kind = "ExternalOutput" if name in debug else "Internal"
        return nc.dram_tensor(name, list(shape), dt, kind=kind).ap()

    x_in = din("x", [T, D])
    mem_in = din("mem", [256, D])
    table = din("rel_bias_table", [32, 28])
    norm_g = din("norm_g", [4, 6, D])
    ev_w_in = din("ev_w_in", [2, D, EV_COLS])
    ev_a_kv_norm = din("ev_a_kv_norm", [2, 128])
    ev_a_w_uk = din("ev_a_w_uk", [2, 8, 64, 128])
    ev_a_w_uv = din("ev_a_w_uv", [2, 8, 128, 64])
    ev_b_lambda = din("ev_b_lambda", [2, 4, 64])
    ev_b_subln = din("ev_b_subln", [2, 128])
    ev_w_out = din("ev_w_out", [2, D, D])
    od_w_in = din("od_w_in", [2, D, OD_COLS])
    od_sinks = din("od_sinks", [2, 16])
    od_w_out = din("od_w_out", [2, D, D])
    xa_wq = din("xa_wq", [4, D, 256])
    xa_wkv = din("xa_wkv", [4, D, 512])
    xa_wo = din("xa_wo", [4, 256, D])
    xa_mem_norm = din("xa_mem_norm", [4, D])
    mlp_w1 = din("mlp_w1", [4, D, 4096])
    mlp_w2 = din("mlp_w2", [4, 4096, D])
    oh_in = din("c_oh", [32, 6 * 16384])
    c128_in = din("c_128", [128, 272])
    hmask_in = din("c_hmask", [28, 1])
    out = nc.dram_tensor("out", [T, D], F32, kind="ExternalOutput").ap()

    ev_w_in_b = dtmp("ev_w_in_b", [2, D, EV_COLS], BF16)
    ev_w_out_b = dtmp("ev_w_out_b", [2, D, D], BF16)
    od_w_in_b = dtmp("od_w_in_b", [2, D, OD_COLS], BF16)
    od_w_out_b = dtmp("od_w_out_b", [2, D, D], BF16)
    wuk_b = dtmp("wuk_b", [2, 8, 64, 128], BF16)
    wuv_b = dtmp("wuv_b", [2, 8, 128, 64], BF16)
    wq_b = dtmp("wq_b", [4, D, 256], BF16)
    wkv_b = dtmp("wkv_b", [4, D, 512], BF16)
    wo_b = dtmp("wo_b", [4, 256, D], BF16)
    w1_b = dtmp("w1_b", [4, D, 4096], BF16)
    w2_b = dtmp("w2_b", [4, 4096, D], BF16)
    Bscr = dtmp("Bscr", [28, 6, 128, 128], BF16)
    KXT = dtmp("KXT", [4, 64, 4, 256], BF16)
    VX = dtmp("VX", [4, 256, 256], BF16)
    QLT = dtmp("QLT", [8, 128, T], BF16)
    QIT = dtmp("QIT", [8, 64, T], BF16)
    WIX = dtmp("WIX", [T, 8], F32)
    QBT = dtmp("QBT", [8, 64, T], BF16)
    CKV = dtmp("CKV", [T, 128], BF16)
    CKVT = dtmp("CKVT", [128, T], BF16)
    KIT = dtmp("KIT", [64, T], BF16)
    KBT = dtmp("KBT", [8, 64, T], BF16)
    VB = dtmp("VB", [T, 512], BF16)
    OT8 = dtmp("OT8", [8, 128, T], BF16)
    QT = dtmp("QT", [16, 64, T], BF16)
    KT = dtmp("KT", [2, 64, T], BF16)
    VV = dtmp("VV", [T, 128], BF16)
    OT16 = dtmp("OT16", [16, 64, T], BF16)

    with ExitStack() as es:
        k = K(nc, es)
        op, pe, dma = k.op, k.pe, k.dma
        P = k.P

        def gsb(name, shape, dt):
            t = es.enter_context(nc.sbuf_tensor(name, shape, dt))
            return Buf(name, t)

        identf = gsb("identf", [128, 128], F32)
        identb = gsb("identb", [128, 128], BF16)
        onesb = gsb("onesb", [128, 128], BF16)
        onesf = gsb("onesf", [128, 128], F32)
        epsb = gsb("epsb", [128, 1], F32)
        cneg = gsb("cneg", [128, 128], F32)
        pow2 = gsb("pow2", [128, 16], F32)
        neglam = gsb("neglam", [128, 2], F32)
        gsub = gsb("gsub", [128, 2], F32)

        def done(name):
            return stop == name

        with Stage(k) as st:
            dma("sync", pow2.t[:], c128_in[:, 0:16], writes=[pow2])
            dma("sync", cneg.t[:], c128_in[:, 16:144], writes=[cneg])
            dma("sync", identf.t[:], c128_in[:, 144:272], writes=[identf])
            op("vector", lambda e: e.tensor_copy(out=identb.t[:], in_=identf.t[:]), [identf], [identb])
            op("vector", lambda e: e.memset(onesb.t[:], 1.0), [], [onesb])
            op("vector", lambda e: e.memset(onesf.t[:], 1.0), [], [onesf])
            op("vector", lambda e: e.memset(epsb.t[:], EPS), [], [epsb])
            castb = Buf("castb")
            if stop == "s0a":
                k.barrier()
                return nc

            def cast2d(dst, src, rows):
                for r0 in range(0, rows, 256):
                    r1 = min(rows, r0 + 256)
                    dma("gpsimd", dst[r0:r1, :], src[r0:r1, :], sembuf=castb)
            for e_ in range(2):
                cast2d(ev_w_in_b[e_], ev_w_in[e_], D)
                cast2d(ev_w_out_b[e_], ev_w_out[e_], D)
                cast2d(od_w_in_b[e_], od_w_in[e_], D)
                cast2d(od_w_out_b[e_], od_w_out[e_], D)
                cast2d(wuk_b[e_].rearrange("h d l -> (h d) l"), ev_a_w_uk[e_].rearrange("h d l -> (h d) l"), 512)
                cast2d(wuv_b[e_].rearrange("h l d -> (h l) d"), ev_a_w_uv[e_].rearrange("h l d -> (h l) d"), 1024)
            for l in range(4):
                cast2d(wq_b[l], xa_wq[l], D)
                cast2d(wkv_b[l], xa_wkv[l], D)
                cast2d(wo_b[l], xa_wo[l], 256)
                cast2d(w1_b[l], mlp_w1[l], D)
                cast2d(w2_b[l], mlp_w2[l], 4096)
            lrow = st.sb("lrow", [1, 2, 256], F32)
            prod = st.sb("prod", [1, 2, 2, 64], F32)
            ssum = st.sb("ssum", [1, 4], F32)
            esum = st.sb("esum", [1, 4], F32)
            nlr = st.sb("nlr", [1, 2], F32)
            sub_t = st.sb("sub_t", [128, 2], F32)
            for e_ in range(2):
                dma("sync", lrow.t[0:1, e_, :], ev_b_lambda[e_:e_ + 1, :, :].rearrange("o a b -> o (a b)"), writes=[lrow])
                dma("sync", sub_t.t[:, e_:e_ + 1], ev_b_subln[e_:e_ + 1, :].rearrange("o p -> p o"), writes=[sub_t])
            for e_ in range(2):
                lam_init = 0.8 - 0.6 * math.exp(-0.3 * (2 * e_))
                for j in range(2):
                    op("vector", lambda e: e.tensor_tensor(out=prod.t[0:1, e_, j, :], in0=lrow.t[0:1, e_, (2 * j) * 64:(2 * j + 1) * 64],
                                                           in1=lrow.t[0:1, e_, (2 * j + 1) * 64:(2 * j + 2) * 64], op=ALU.mult), [lrow], [prod])
                    op("vector", lambda e: e.tensor_reduce(out=ssum.t[0:1, 2 * e_ + j:2 * e_ + j + 1], in_=prod.t[0:1, e_, j, :], axis=AX.X, op=ALU.add), [prod], [ssum])
                op("scalar", lambda e: e.activation(out=esum.t[0:1, 2 * e_:2 * e_ + 2], in_=ssum.t[0:1, 2 * e_:2 * e_ + 2], func=AF.Exp), [ssum], [esum])
                op("vector", lambda e: e.tensor_tensor(out=nlr.t[0:1, e_:e_ + 1], in0=esum.t[0:1, 2 * e_ + 1:2 * e_ + 2], in1=esum.t[0:1, 2 * e_:2 * e_ + 1], op=ALU.subtract), [esum], [nlr])
                op("vector", lambda e: e.tensor_scalar(out=nlr.t[0:1, e_:e_ + 1], in0=nlr.t[0:1, e_:e_ + 1], scalar1=-lam_init, scalar2=None, op0=ALU.add), [nlr], [nlr])
                op("vector", lambda e: e.tensor_scalar(out=gsub.t[:, e_:e_ + 1], in0=sub_t.t[:, e_:e_ + 1], scalar1=(1.0 - lam_init), scalar2=None, op0=ALU.mult), [sub_t], [gsub])
            pe([lambda e: e.matmul(k.pb(0)[:, 0:2], lhsT=onesf.t[0:1, :], rhs=nlr.t[0:1, :], start=True, stop=True)], [onesf, nlr], [P[0]])
            op("vector", lambda e: e.tensor_copy(out=neglam.t[:], in_=k.pb(0)[:, 0:2]), [P[0]], [neglam])

        with Stage(k) as st:
            tab = st.sb("tab", [32, 28], F32)
            cm = st.sb("cm", [28, 1], F32)
            hm = st.sb("hm", [28, 1], F32)
            dma("sync", tab.t[:], table[:, :], writes=[tab])
            dma("sync", cm.t[:], table[15:16, :].rearrange("o h -> h o"), writes=[cm])
            dma("sync", hm.t[:], hmask_in[:, :], writes=[hm])
            op("vector", lambda e: e.tensor_tensor(out=cm.t[:], in0=cm.t[:], in1=hm.t[:], op=ALU.mult), [hm], [cm])
            ohb = [st.sb(f"ohb{i}", [32, 4096], F32) for i in range(2)]
            bsb = [st.sb(f"bsb{i}", [28, 4096], BF16) for i in range(2)]
            Bflat = Bscr.rearrange("h d s q -> h (d s q)")
            for i in range(24):
                o_ = ohb[i % 2]
                b_ = bsb[i % 2]
                dma("sync", o_.t[:], oh_in[:, i * 4096:(i + 1) * 4096], writes=[o_])
                for j in range(8):
                    pk = (i * 8 + j) % 4
                    pe([lambda e: e.matmul(k.pb(pk)[0:28, :], lhsT=tab.t[:, :], rhs=o_.t[:, j * 512:(j + 1) * 512], start=True, stop=True)], [tab, o_], [P[pk]])
                    op("vector", lambda e: e.tensor_scalar(out=b_.t[:, j * 512:(j + 1) * 512], in0=k.pb(pk)[0:28, :], scalar1=cm.t[:, 0:1], scalar2=8.0, op0=ALU.subtract, op1=ALU.mult), [P[pk], cm], [b_])
                dma("gpsimd", Bflat[:, i * 4096:(i + 1) * 4096], b_.t[:], reads=[b_])
        with Stage(k) as st:
            negt = st.sb("negt", [28, 64, 64], BF16)
            op("vector", lambda e: e.memset(negt.t[:], NEG), [], [negt])
            dma("gpsimd", Bscr[:, 0, 64:128, 0:64], negt.t[:], reads=[negt])

        with Stage(k) as st:
            mt = [st.sb(f"mt{i}", [128, D], F32) for i in range(2)]
            for i in range(2):
                dma("sync", mt[i].t[:], mem_in[i * 128:(i + 1) * 128, :], writes=[mt[i]])
            junk = st.sb("junk", [128, D], BF16)
            ss = st.sb("ss", [128, 2], F32)
            rstd = st.sb("rstd", [128, 2], F32)
            for i in range(2):
                op("scalar", lambda e: e.activation(out=junk.t[:], in_=mt[i].t[:], func=AF.Square, accum_out=ss.t[:, i:i + 1]), [mt[i]], [junk, ss])
            op("scalar", lambda e: e.activation(out=rstd.t[:], in_=ss.t[:], func=AF.Sqrt, bias=epsb.t[:, 0:1], scale=1.0 / D), [ss, epsb], [rstd])
            op("vector", lambda e: e.reciprocal(out=rstd.t[:], in_=rstd.t[:]), [rstd], [rstd])
            gm = st.sb("gm", [128, D], F32)
            mb = st.sb("mb", [128, D], BF16)
            memT = st.sb("memT", [128, 8, 256], BF16)
            wkv = st.sb("wkv", [128, 8, 512], BF16)
            kx = st.sb("kx", [64, 4, 256], BF16)
            vx = st.sb("vx", [128, 2, 256], BF16)
            for l in range(4):
                dma("sync", gm.t[:], xa_mem_norm[l:l + 1, :].partition_broadcast(128), writes=[gm])
                dma("sync", wkv.t[:], wkv_b[l].rearrange("(kc p) n -> p kc n", p=128), writes=[wkv])
                for i in range(2):
                    op("vector", lambda e: e.scalar_tensor_tensor(out=mb.t[:], in0=mt[i].t[:], scalar=rstd.t[:, i:i + 1], in1=gm.t[:], op0=ALU.mult, op1=ALU.mult), [mt[i], rstd, gm], [mb])
                    pe([(lambda e, kc=kc: e.transpose(out=k.pbb(0)[:, kc * 128:(kc + 1) * 128], in_=mb.t[:, kc * 128:(kc + 1) * 128], identity=identb.t[:])) for kc in range(8)], [mb, identb], [P[0]])
                    op("vector", lambda e: e.tensor_copy(out=memT.t[:, :, i * 128:(i + 1) * 128], in_=k.pbb(0).rearrange("p (a b) -> p a b", a=8)), [P[0]], [memT])
                for h in range(4):
                    pe([(lambda e, kc=kc: e.matmul(k.pb(1)[0:64, 0:256], lhsT=wkv.t[:, kc, h * 64:(h + 1) * 64], rhs=memT.t[:, kc, :], start=(kc == 0), stop=(kc == 7))) for kc in range(8)], [wkv, memT], [P[1]])
                    op("vector", lambda e: e.tensor_copy(out=kx.t[:, h, :], in_=k.pb(1)[0:64, 0:256]), [P[1]], [kx])
                for i in range(2):
                    pe([(lambda e, kc=kc: e.matmul(k.pb(2)[:, 0:256], lhsT=memT.t[:, kc, i * 128:(i + 1) * 128], rhs=wkv.t[:, kc, 256:512], start=(kc == 0), stop=(kc == 7))) for kc in range(8)], [wkv, memT], [P[2]])
                    op("vector", lambda e: e.tensor_copy(out=vx.t[:, i, :], in_=k.pb(2)[:, 0:256]), [P[2]], [vx])
                dma("gpsimd", KXT[l], kx.t[:], reads=[kx])
                dma("gpsimd", VX[l].rearrange("(i p) n -> p i n", p=128), vx.t[:], reads=[vx])

        def norm_to_hT(st, xt_list, gbc, hb, hT, junk, ss, rstd, pbank):
            n = len(xt_list)
            for t_ in range(n):
                op("scalar", lambda e: e.activation(out=junk.t[:], in_=xt_list[t_].t[:], func=AF.Square, accum_out=ss.t[:, t_:t_ + 1]), [xt_list[t_]], [junk, ss])
            op("scalar", lambda e: e.activation(out=rstd.t[:, 0:n], in_=ss.t[:, 0:n], func=AF.Sqrt, bias=epsb.t[:, 0:1], scale=1.0 / D), [ss, epsb], [rstd])
            op("vector", lambda e: e.reciprocal(out=rstd.t[:, 0:n], in_=rstd.t[:, 0:n]), [rstd], [rstd])
            for t_ in range(n):
                op("vector", lambda e: e.scalar_tensor_tensor(out=hb.t[:], in0=xt_list[t_].t[:], scalar=rstd.t[:, t_:t_ + 1], in1=gbc.t[:], op0=ALU.mult, op1=ALU.mult), [xt_list[t_], rstd, gbc], [hb])
                pk = pbank[t_ % len(pbank)]
                pe([(lambda e, kc=kc: e.transpose(out=k.pbb(pk)[:, kc * 128:(kc + 1) * 128], in_=hb.t[:, kc * 128:(kc + 1) * 128], identity=identb.t[:])) for kc in range(8)], [hb, identb], [P[pk]])
                k.copy(k.evac_eng(), hT.t[:, :, t_ * 128:(t_ + 1) * 128], k.pbb(pk).rearrange("p (a b) -> p a b", a=8), [P[pk]], [hT])

        pn_a = gsb("pn_a", [128, 4], F32)
        pn_b = gsb("pn_b", [128, 4], F32)

        def post_norm_add(st, xt_list, ysb_list, sspart, gbc, rstd, tmp):
            n = len(xt_list)
            ssv = sspart.t[:, 0:2 * n].rearrange("p (t h) -> p t h", h=2)
            op("vector", lambda e: e.tensor_reduce(out=pn_a.t[:, 0:n], in_=ssv, axis=AX.X, op=ALU.add), [sspart], [pn_a])
            op("scalar", lambda e: e.activation(out=pn_b.t[:, 0:n], in_=pn_a.t[:, 0:n], func=AF.Sqrt, bias=epsb.t[:, 0:1], scale=1.0 / D), [pn_a, epsb], [pn_b])
            op("vector", lambda e: e.reciprocal(out=rstd.t[:, 0:n], in_=pn_b.t[:, 0:n]), [pn_b], [rstd])
            for t_ in range(n):
                op("vector", lambda e: e.scalar_tensor_tensor(out=tmp.t[:], in0=ysb_list[t_].t[:], scalar=rstd.t[:, t_:t_ + 1], in1=gbc.t[:], op0=ALU.mult, op1=ALU.mult), [ysb_list[t_], rstd, gbc], [tmp])
                op("vector", lambda e: e.tensor_tensor(out=xt_list[t_].t[:], in0=xt_list[t_].t[:], in1=tmp.t[:], op=ALU.add), [tmp], [xt_list[t_]])

        def evac_y(pk, ysb, half, sspart, t_, junk):
            op("vector", lambda e: e.tensor_copy(out=ysb.t[:, half * 512:(half + 1) * 512], in_=k.pb(pk)), [P[pk]], [ysb])
            op("scalar", lambda e: e.activation(out=junk.t[:, 0:512], in_=k.pb(pk), func=AF.Square, accum_out=sspart.t[:, 2 * t_ + half:2 * t_ + half + 1]), [P[pk]], [junk, sspart])

        def proj_even(l):
            e_ = l // 2
            xsrc = x_in if l == 0 else out
            with Stage(k) as st:
                win = st.sb("win", [128, 8, EV_COLS], BF16)
                for kc in range(8):
                    dma("sync", win.t[:, kc, :], ev_w_in_b[e_][kc * 128:(kc + 1) * 128, :], writes=[win])
                wuk = st.sb("wuk", [128, 4, 128], BF16)
                dma("sync", wuk.t[:], wuk_b[e_].rearrange("(hp par) d l -> (par d) hp l", par=2), writes=[wuk])
                g0 = st.sb("g0", [128, D], F32)
                dma("sync", g0.t[:], norm_g[l, 0:1, :].partition_broadcast(128), writes=[g0])
                akv = st.sb("akv", [128, 128], F32)
                dma("sync", akv.t[:], ev_a_kv_norm[e_:e_ + 1, :].partition_broadcast(128), writes=[akv])
                xt = [st.sb(f"xt{i}", [128, D], F32) for i in range(4)]
                hb = st.sb("hb", [128, D], BF16)
                junk = st.sb("junk", [128, D], BF16)
                hT = st.sb("hT", [128, 8, 512], BF16)
                ss = st.sb("ss", [128, 4], F32)
                rstd = st.sb("rstd", [128, 4], F32)
                qaT = st.sb("qaT", [128, 4, 512], BF16)
                fm = [st.sb(f"fm{i}", [128, 512], BF16) for i in range(3)]
                ssc = st.sb("ssc", [128, 1], F32)
                rsc = st.sb("rsc", [128, 1], F32)
                ckv_sb = [st.sb(f"ckv_sb{i}", [128, 128], BF16) for i in range(2)]
                ckvT_sb = [st.sb(f"ckvT_sb{i}", [128, 128], BF16) for i in range(2)]
                wi_sb = [st.sb(f"wi_sb{i}", [128, 8], F32) for i in range(2)]
                vb_sb = [st.sb(f"vb_sb{i}", [128, 512], BF16) for i in range(2)]
                fmi = 0
                for g in range(NGR):
                    tok0 = g * 512
                    for t_ in range(4):
                        dma("sync", xt[t_].t[:], xsrc[tok0 + t_ * 128:tok0 + (t_ + 1) * 128, :], writes=[xt[t_]])
                    norm_to_hT(st, xt, g0, hb, hT, junk, ss, rstd, [0, 1])
                    blocks = []
                    for p_ in range(4):
                        blocks.append((p_ * 128, 128, ("qa", p_)))
                    for p_ in range(4):
                        blocks.append((640 + p_ * 128, 128, ("st", QIT[2 * p_:2 * p_ + 2].rearrange("a d t -> (a d) t"))))
                    blocks.append((1152, 64, ("st", KIT)))
                    for p_ in range(4):
                        blocks.append((1224 + p_ * 128, 128, ("st", QBT[2 * p_:2 * p_ + 2].rearrange("a d t -> (a d) t"))))
                    for p_ in range(4):
                        blocks.append((1736 + p_ * 128, 128, ("st", KBT[2 * p_:2 * p_ + 2].rearrange("a d t -> (a d) t"))))
                    for bi, (c0, ncol, dest) in enumerate(blocks):
                        pk = 2 + bi % 2
                        pe([(lambda e, kc=kc: e.matmul(k.pb(pk)[0:ncol, :], lhsT=win.t[:, kc, c0:c0 + ncol], rhs=hT.t[:, kc, :], start=(kc == 0), stop=(kc == 7))) for kc in range(8)], [win, hT], [P[pk]])
                        if dest[0] == "qa":
                            k.copy(k.evac_eng(), qaT.t[:, dest[1], :], k.pb(pk), [P[pk]], [qaT])
                        else:
                            f_ = fm[fmi % 3]
                            fmi += 1
                            k.copy(k.evac_eng(), f_.t[0:ncol, :], k.pb(pk)[0:ncol, :], [P[pk]], [f_])
                            dma("gpsimd", dest[1][:, tok0:tok0 + 512], f_.t[0:ncol, :], reads=[f_])
                    for h in range(8):
                        pk = 2 + h % 2
                        b0 = (h % 2) * 64
                        pe([lambda e: e.matmul(k.pb(pk), lhsT=wuk.t[b0:b0 + 64, h // 2, :], rhs=qaT.t[b0:b0 + 64, h // 2, :], start=True, stop=True)], [wuk, qaT], [P[pk]])
                        f_ = fm[fmi % 3]
                        fmi += 1
                        k.copy(k.evac_eng(), f_.t[:], k.pb(pk), [P[pk]], [f_])
                        dma("gpsimd", QLT[h, :, tok0:tok0 + 512], f_.t[:], reads=[f_])
                    for t_ in range(4):
                        r0 = tok0 + t_ * 128
                        i2 = t_ % 2
                        pe([(lambda e, kc=kc: e.matmul(k.pb(4)[:, 0:128], lhsT=hT.t[:, kc, t_ * 128:(t_ + 1) * 128], rhs=win.t[:, kc, 512:640], start=(kc == 0), stop=(kc == 7))) for kc in range(8)]
                           + [(lambda e, kc=kc: e.matmul(k.pb(4)[:, 128:136], lhsT=hT.t[:, kc, t_ * 128:(t_ + 1) * 128], rhs=win.t[:, kc, 1216:1224], start=(kc == 0), stop=(kc == 7))) for kc in range(8)], [win, hT], [P[4]])
                        pe([(lambda e, kc=kc: e.matmul(k.pb(5), lhsT=hT.t[:, kc, t_ * 128:(t_ + 1) * 128], rhs=win.t[:, kc, 2248:2760], start=(kc == 0), stop=(kc == 7))) for kc in range(8)], [win, hT], [P[5]])
                        op("scalar", lambda e: e.activation(out=junk.t[:, 0:128], in_=k.pb(4)[:, 0:128], func=AF.Square, accum_out=ssc.t[:, 0:1]), [P[4]], [junk, ssc])
                        op("scalar", lambda e: e.activation(out=rsc.t[:], in_=ssc.t[:], func=AF.Sqrt, bias=epsb.t[:, 0:1], scale=1.0 / 128), [ssc, epsb], [rsc])
                        op("vector", lambda e: e.reciprocal(out=rsc.t[:], in_=rsc.t[:]), [rsc], [rsc])
                        op("vector", lambda e: e.scalar_tensor_tensor(out=ckv_sb[i2].t[:], in0=k.pb(4)[:, 0:128], scalar=rsc.t[:, 0:1], in1=akv.t[:], op0=ALU.mult, op1=ALU.mult), [P[4], rsc, akv], [ckv_sb[i2]])
                        op("vector", lambda e: e.tensor_copy(out=wi_sb[i2].t[:], in_=k.pb(4)[:, 128:136]), [P[4]], [wi_sb[i2]])
                        dma("gpsimd", CKV[r0:r0 + 128, :], ckv_sb[i2].t[:], reads=[ckv_sb[i2]])
                        dma("gpsimd", WIX[r0:r0 + 128, :], wi_sb[i2].t[:], reads=[wi_sb[i2]])
                        pe([lambda e: e.transpose(out=k.pbb(6)[:, 0:128], in_=ckv_sb[i2].t[:], identity=identb.t[:])], [ckv_sb[i2], identb], [P[6]])
                        op("vector", lambda e: e.tensor_copy(out=ckvT_sb[i2].t[:], in_=k.pbb(6)[:, 0:128]), [P[6]], [ckvT_sb[i2]])
                        dma("gpsimd", CKVT[:, r0:r0 + 128], ckvT_sb[i2].t[:], reads=[ckvT_sb[i2]])
                        op("scalar", lambda e: e.copy(out=vb_sb[i2].t[:], in_=k.pb(5)), [P[5]], [vb_sb[i2]])
                        dma("gpsimd", VB[r0:r0 + 128, :], vb_sb[i2].t[:], reads=[vb_sb[i2]])

        def attn_dsa(l):
            e_ = l // 2
            with Stage(k) as st:
                ckvT = st.sb("ckvT", [128, T], BF16)
                ckv = st.sb("ckv", [128, 64, 128], BF16)
                kiT = st.sb("kiT", [64, T], BF16)
                for c0 in range(0, NTR * 128, 2048):
                    c1 = min(NTR * 128, c0 + 2048)
                    dma("sync", ckvT.t[:, c0:c1], CKVT[:, c0:c1], writes=[ckvT])
                    dma("sync", kiT.t[:, c0:c1], KIT[:, c0:c1], writes=[kiT])
                    dma("sync", ckv.t[:, c0 // 128:c1 // 128, :], CKV[c0:c1, :].rearrange("(j p) l -> p j l", p=128), writes=[ckv])
                wuv = st.sb("wuv", [128, 8, 64], BF16)
                dma("sync", wuv.t[:], wuv_b[e_].rearrange("h l d -> l h d"), writes=[wuv])
                biasA = st.sb("biasA", [128, 6, 8, 128], BF16)
                for d in range(6):
                    dma("sync", biasA.t[:, d, :, :], Bscr[0:8, d, :, :].rearrange("h s q -> s h q"), writes=[biasA])
                sel8 = st.sb("sel8", [128, 8, 128], BF16)
                for h in range(8):
                    op("vector", lambda e: e.tensor_copy(out=sel8.t[:, h, :], in_=identb.t[:]), [identb], [sel8])
                I = st.sb("I", [128, T], F32)
                junk = st.sb("junk", [128, 2048], BF16)
                negm = [st.sb(f"negm{i}", [128, T], BF16) for i in range(2)]
                qiT = [st.sb(f"qiT{i}", [64, 8, 128], BF16) for i in range(2)]
                qlT = [st.sb(f"qlT{i}", [128, 8, 128], BF16) for i in range(2)]
                wi = [st.sb(f"wi{i}", [128, 8], F32) for i in range(2)]
                arel = [st.sb(f"arel{i}", [128, 512], BF16) for i in range(4)]
                absw = [st.sb(f"absw{i}", [128, 8], F32) for i in range(2)]
                sgw = [st.sb(f"sgw{i}", [128, 8], F32) for i in range(2)]
                Dg = [st.sb(f"Dg{i}", [128, 8, 128], BF16) for i in range(2)]
                amp = st.sb("amp", [128, 16], F32)
                amax = st.sb("amax", [128, 1], F32)
                lo = st.sb("lo", [128, 1], F32)
                mid = st.sb("mid", [128, 1], F32)
                cnt = st.sb("cnt", [128, 1], F32)
                tmp1 = st.sb("tmp1", [128, 1], F32)
                wall = st.sb("wall", [128, 16], F32)
                PT = [st.sb(f"PT{i}", [128, 512], BF16) for i in range(4)]
                rz = st.sb("rz", [128, 1024], F32)
                oaT = st.sb("oaT", [128, 8, 128], BF16)
                oub = [st.sb(f"oub{i}", [128, 4, 128], BF16) for i in range(2)]
                scale = 64 ** -0.5
                aic = [0]
                ptc = [0]

                def idx_phase(qi):
                    ai = aic[0]
                    tok0 = qi * 128
                    b2 = qi % 2
                    dma("sync", qiT[b2].t[:], QIT[:, :, tok0:tok0 + 128].rearrange("h d t -> d h t"), writes=[qiT[b2]])
                    dma("sync", qlT[b2].t[:], QLT[:, :, tok0:tok0 + 128].rearrange("h l t -> l h t"), writes=[qlT[b2]])
                    dma("sync", wi[b2].t[:], WIX[tok0:tok0 + 128, :], writes=[wi[b2]])
                    nkeys = (qi + 1) * 128
                    nch = (nkeys + 511) // 512
                    wb_ = wi[b2]
                    op("vector", lambda e: e.tensor_scalar(out=sgw[b2].t[:], in0=wb_.t[:], scalar1=0.0, scalar2=2.0, op0=ALU.is_ge, op1=ALU.mult), [wb_], [sgw[b2]])
                    op("vector", lambda e: e.tensor_scalar(out=sgw[b2].t[:], in0=sgw[b2].t[:], scalar1=-1.0, scalar2=None, op0=ALU.add), [], [sgw[b2]])
                    op("vector", lambda e: e.tensor_tensor(out=absw[b2].t[:], in0=wb_.t[:], in1=sgw[b2].t[:], op=ALU.mult), [wb_, sgw[b2]], [absw[b2]])
                    for h in range(8):
                        op("vector", lambda e: e.tensor_scalar(out=Dg[b2].t[:, h, :], in0=identb.t[:], scalar1=sgw[b2].t[:, h:h + 1], scalar2=None, op0=ALU.mult), [identb, sgw[b2]], [Dg[b2]])
                    for c in range(nch):
                        c0 = c * 512
                        cw = min(512, nkeys - c0)
                        alist = []

                        def dmm(h):
                            a__ = alist[h]
                            pe([lambda e: e.matmul(k.pb(3)[:, 0:cw], lhsT=Dg[b2].t[:, h, :], rhs=a__.t[:, 0:cw], start=(h == 0), stop=(h == 7))], [Dg[b2], a__], [P[3]])
                        for h in range(8):
                            pk = h % 2
                            pe([lambda e: e.matmul(k.pb(pk)[:, 0:cw], lhsT=qiT[b2].t[:, h, :], rhs=kiT.t[:, c0:c0 + cw], start=True, stop=True)], [qiT[b2], kiT], [P[pk]])
                            a_ = arel[ai % 4]
                            ai += 1
                            alist.append(a_)
                            op("scalar", lambda e: e.activation(out=a_.t[:, 0:cw], in_=k.pb(pk)[:, 0:cw], func=AF.Relu, scale=absw[b2].t[:, h:h + 1]), [P[pk], absw[b2]], [a_])
                            if h >= 1:
                                dmm(h - 1)
                        dmm(7)
                        op("vector", lambda e: e.tensor_copy(out=I.t[:, c0:c0 + cw], in_=k.pb(3)[:, 0:cw]), [P[3]], [I])
                        op("vector", lambda e: e.tensor_reduce(out=amp.t[:, c:c + 1], in_=I.t[:, c0:c0 + cw], axis=AX.X, op=ALU.max, apply_absolute_value=True), [I], [amp])
                    op("vector", lambda e: e.tensor_tensor(out=I.t[:, tok0:tok0 + 128], in0=I.t[:, tok0:tok0 + 128], in1=cneg.t[:], op=ALU.add), [cneg], [I])
                    op("vector", lambda e: e.tensor_reduce(out=amax.t[:], in_=amp.t[:, 0:nch], axis=AX.X, op=ALU.max), [amp], [amax])
                    op("vector", lambda e: e.tensor_scalar(out=lo.t[:], in0=amax.t[:], scalar1=-1.0, scalar2=None, op0=ALU.mult), [amax], [lo])
                    op("vector", lambda e: e.tensor_scalar(out=wall.t[:], in0=pow2.t[:], scalar1=amax.t[:, 0:1], scalar2=None, op0=ALU.mult), [pow2, amax], [wall])
                    if nkeys > 256:
                        for it in range(NBIS):
                            op("vector", lambda e: e.tensor_tensor(out=mid.t[:], in0=lo.t[:], in1=wall.t[:, it:it + 1], op=ALU.add), [lo, wall], [mid])
                            for a0 in range(0, nkeys, 2048):
                                a1 = min(nkeys, a0 + 2048)
                                first = (a0 == 0)
                                op("vector", lambda e: e.tensor_scalar(out=junk.t[:, 0:a1 - a0], in0=I.t[:, a0:a1], scalar1=mid.t[:, 0:1], scalar2=(None if first else cnt.t[:, 0:1]),
                                                                       op0=ALU.is_ge, op1=ALU.add, accum_out=cnt.t[:, 0:1]), [I, mid] + ([] if first else [cnt]), [junk, cnt])
                            op("vector", lambda e: e.scalar_tensor_tensor(out=tmp1.t[:], in0=cnt.t[:], scalar=255.5, in1=wall.t[:, it:it + 1], op0=ALU.is_ge, op1=ALU.mult), [cnt, wall], [tmp1])
                            op("vector", lambda e: e.tensor_tensor(out=lo.t[:], in0=lo.t[:], in1=tmp1.t[:], op=ALU.add), [tmp1], [lo])
                    nm = negm[b2]
                    op("vector", lambda e: e.tensor_scalar(out=nm.t[:, 0:nkeys], in0=I.t[:, 0:nkeys], scalar1=lo.t[:, 0:1], scalar2=NEG, op0=ALU.is_lt, op1=ALU.mult), [I, lo], [nm])
                    aic[0] = ai

                def dsa_phase(qi):
                    tok0 = qi * 128
                    b2 = qi % 2
                    nm = negm[b2]
                    items = [(j, half) for j in range(qi + 1) for half in range(2)]
                    pts = {}

                    def pvz(idx):
                        j, half = items[idx]
                        pt_ = pts.pop(idx)
                        pe([lambda e: e.matmul(k.pb(4 + half), lhsT=ckv.t[:, j, :], rhs=pt_.t[:], start=(j == 0), stop=(j == qi)),
                            lambda e: e.matmul(k.pb(6 + half), lhsT=onesb.t[:], rhs=pt_.t[:], start=(j == 0), stop=(j == qi))], [ckv, pt_, onesb], [P[4 + half], P[6 + half]])
                    for idx, (j, half) in enumerate(items):
                        d = qi - j
                        near = d <= 5
                        fns = [lambda e: e.matmul(k.pb(2), lhsT=ckvT.t[:, j * 128:(j + 1) * 128], rhs=qlT[b2].t[:, half * 4:(half + 1) * 4, :], start=True, stop=False),
                               lambda e: e.matmul(k.pb(2), lhsT=nm.t[:, j * 128:(j + 1) * 128], rhs=sel8.t[:, 0:4, :], start=False, stop=(not near))]
                        if near:
                            fns.append(lambda e: e.matmul(k.pb(2), lhsT=identb.t[:], rhs=biasA.t[:, d, half * 4:(half + 1) * 4, :], start=False, stop=True))
                        pe(fns, [ckvT, qlT[b2], nm, sel8, identb, biasA], [P[2]])
                        pt = PT[ptc[0] % 4]
                        ptc[0] += 1
                        pts[idx] = pt
                        op("scalar", lambda e: e.activation(out=pt.t[:], in_=k.pb(2), func=AF.Exp, scale=scale), [P[2]], [pt])
                        if idx >= 1:
                            pvz(idx - 1)
                    pvz(len(items) - 1)
                    op("vector", lambda e: e.reciprocal(out=rz.t[:], in_=k.pb2(3)), [P[6], P[7]], [rz])
                    op("vector", lambda e: e.tensor_tensor(out=oaT.t[:].rearrange("p a b -> p (a b)"), in0=k.pb2(2), in1=rz.t[:], op=ALU.mult), [P[4], P[5], rz], [oaT])
                    fns = []
                    for h in range(8):
                        fns.append(lambda e, h=h: e.matmul(k.pb(0)[(h % 2) * 64:(h % 2) * 64 + 64, (h // 2) * 128:(h // 2 + 1) * 128], lhsT=wuv.t[:, h, :], rhs=oaT.t[:, h, :], start=True, stop=True))
                    pe(fns, [wuv, oaT], [P[0]])
                    ob = oub[b2]
                    op("vector", lambda e: e.tensor_copy(out=ob.t[:].rearrange("p a b -> p (a b)"), in_=k.pb(0)), [P[0]], [ob])
                    dma("gpsimd", OT8[0:4, :, tok0:tok0 + 128].rearrange("b p t -> p b t"), ob.t[:], reads=[ob])

                idx_phase(0)
                for qi in range(NTR):
                    if qi + 1 < NTR:
                        idx_phase(qi + 1)
                    dsa_phase(qi)

        def attn_diff(l):
            e_ = l // 2
            scale = 64 ** -0.5
            with Stage(k) as st:
                kbT = st.sb("kbT", [64, 2, T], BF16)
                vbh = st.sb("vbh", [128, 64, 128], BF16)
                dbias = st.sb("dbias", [128, 9, 4, 128], BF16)
                qbT = [st.sb(f"qbT{i}", [64, 512], BF16) for i in range(2)]
                PT = [st.sb(f"PT{i}", [128, 512], BF16) for i in range(3)]
                rzz = [st.sb(f"rzz{i}", [128, 512], F32) for i in range(2)]
                aa = [st.sb(f"aa{i}", [128, 512], F32) for i in range(2)]
                oo = st.sb("oo", [128, 512], F32)
                sq = st.sb("sq", [128, 512], F32)
                rs_ = st.sb("rs_", [128, 512], F32)
                obT = [st.sb(f"obT{i}", [128, 512], BF16) for i in range(2)]
                pti = 0
                for h in range(4):
                    for c0 in range(0, NTR * 128, 2048):
                        c1 = min(NTR * 128, c0 + 2048)
                        for m in range(2):
                            dma("sync", kbT.t[:, m, c0:c1], KBT[2 * h + m, :, c0:c1], writes=[kbT])
                        dma("sync", vbh.t[:, c0 // 128:c1 // 128, :], VB[c0:c1, h * 128:(h + 1) * 128].rearrange("(j p) e -> p j e", p=128), writes=[vbh])
                    for ri in range(9):
                        r = ri - 5
                        for t_ in range(4):
                            d = t_ - r
                            if d < 0:
                                op("gpsimd", lambda e: e.memset(dbias.t[:, ri, t_, :], NEG), [], [dbias])
                            elif d <= 5:
                                dma("sync", dbias.t[:, ri, t_, :], Bscr[8 + h, d, :, :], writes=[dbias])
                            else:
                                op("gpsimd", lambda e: e.memset(dbias.t[:, ri, t_, :], 0.0), [], [dbias])
                    for g in range(NGR):
                        tok0 = g * 512
                        nblk = 4 * g + 4
                        for m in range(2):
                            qb = qbT[m]
                            dma("sync", qb.t[:], QBT[2 * h + m, :, tok0:tok0 + 512], writes=[qb])
                            for j in range(nblk):
                                r = j - 4 * g
                                pk = j % 2
                                fns = [lambda e: e.matmul(k.pb(pk), lhsT=kbT.t[:, m, j * 128:(j + 1) * 128], rhs=qb.t[:], start=True, stop=(r < -5))]
                                if r >= -5:
                                    fns.append(lambda e: e.matmul(k.pb(pk), lhsT=identb.t[:], rhs=dbias.t[:, r + 5, :, :], start=False, stop=True))
                                pe(fns, [kbT, qb, identb, dbias], [P[pk]])
                                pt = PT[pti % 3]
                                pti += 1
                                op("scalar", lambda e: e.activation(out=pt.t[:], in_=k.pb(pk), func=AF.Exp, scale=scale), [P[pk]], [pt])
                                pe([lambda e: e.matmul(k.pb(2 + m), lhsT=vbh.t[:, j, :], rhs=pt.t[:], start=(j == 0), stop=(j == nblk - 1)),
                                    lambda e: e.matmul(k.pb(4 + m), lhsT=onesb.t[:], rhs=pt.t[:], start=(j == 0), stop=(j == nblk - 1))], [vbh, pt, onesb], [P[2 + m], P[4 + m]])
                            op("vector", lambda e: e.reciprocal(out=rzz[m].t[:], in_=k.pb(4 + m)), [P[4 + m]], [rzz[m]])
                            op("vector", lambda e: e.tensor_tensor(out=aa[m].t[:], in0=k.pb(2 + m), in1=rzz[m].t[:], op=ALU.mult), [P[2 + m], rzz[m]], [aa[m]])
                        op("vector", lambda e: e.scalar_tensor_tensor(out=oo.t[:], in0=aa[1].t[:], scalar=neglam.t[:, e_:e_ + 1], in1=aa[0].t[:], op0=ALU.mult, op1=ALU.add), [aa[0], aa[1], neglam], [oo])
                        op("scalar", lambda e: e.activation(out=sq.t[:], in_=oo.t[:], func=AF.Square), [oo], [sq])
                        pe([lambda e: e.matmul(k.pb(6), lhsT=onesf.t[:], rhs=sq.t[:], start=True, stop=True)], [onesf, sq], [P[6]])
                        op("scalar", lambda e: e.activation(out=rs_.t[:], in_=k.pb(6), func=AF.Sqrt, bias=epsb.t[:, 0:1], scale=1.0 / 128), [P[6], epsb], [rs_])
                        op("vector", lambda e: e.reciprocal(out=rs_.t[:], in_=rs_.t[:]), [rs_], [rs_])
                        ob = obT[g % 2]
                        op("vector", lambda e: e.scalar_tensor_tensor(out=ob.t[:], in0=oo.t[:], scalar=gsub.t[:, e_:e_ + 1], in1=rs_.t[:], op0=ALU.mult, op1=ALU.mult), [oo, gsub, rs_], [ob])
                        dma("gpsimd", OT8[4 + h, :, tok0:tok0 + 512], ob.t[:], reads=[ob])

        def proj_odd(l):
            o_ = l // 2
            with Stage(k) as st:
                win = st.sb("win", [128, 8, OD_COLS], BF16)
                for kc in range(8):
                    dma("sync", win.t[:, kc, :], od_w_in_b[o_][kc * 128:(kc + 1) * 128, :], writes=[win])
                g0 = st.sb("g0", [128, D], F32)
                dma("sync", g0.t[:], norm_g[l, 0:1, :].partition_broadcast(128), writes=[g0])
                xt = [st.sb(f"xt{i}", [128, D], F32) for i in range(4)]
                hb = st.sb("hb", [128, D], BF16)
                junk = st.sb("junk", [128, D], BF16)
                hT = st.sb("hT", [128, 8, 512], BF16)
                ss = st.sb("ss", [128, 4], F32)
                rstd = st.sb("rstd", [128, 4], F32)
                fm = [st.sb(f"fm{i}", [128, 512], BF16) for i in range(3)]
                v_sb = [st.sb(f"v_sb{i}", [128, 128], BF16) for i in range(2)]
                fmi = 0
                for g in range(NGR):
                    tok0 = g * 512
                    for t_ in range(4):
                        dma("sync", xt[t_].t[:], out[tok0 + t_ * 128:tok0 + (t_ + 1) * 128, :], writes=[xt[t_]])
                    norm_to_hT(st, xt, g0, hb, hT, junk, ss, rstd, [0, 1])
                    blocks = []
                    for p_ in range(8):
                        blocks.append((p_ * 128, QT[2 * p_:2 * p_ + 2].rearrange("a d t -> (a d) t")))
                    blocks.append((1024, KT.rearrange("a d t -> (a d) t")))
                    for bi, (c0, dest) in enumerate(blocks):
                        pk = 2 + bi % 2
                        pe([(lambda e, kc=kc: e.matmul(k.pb(pk), lhsT=win.t[:, kc, c0:c0 + 128], rhs=hT.t[:, kc, :], start=(kc == 0), stop=(kc == 7))) for kc in range(8)], [win, hT], [P[pk]])
                        f_ = fm[fmi % 3]
                        fmi += 1
                        k.copy(k.evac_eng(), f_.t[:], k.pb(pk), [P[pk]], [f_])
                        dma("gpsimd", dest[:, tok0:tok0 + 512], f_.t[:], reads=[f_])
                    for t_ in range(4):
                        r0 = tok0 + t_ * 128
                        pe([(lambda e, kc=kc: e.matmul(k.pb(4)[:, 0:128], lhsT=hT.t[:, kc, t_ * 128:(t_ + 1) * 128], rhs=win.t[:, kc, 1152:1280], start=(kc == 0), stop=(kc == 7))) for kc in range(8)], [win, hT], [P[4]])
                        v_ = v_sb[t_ % 2]
                        k.copy(k.evac_eng(), v_.t[:], k.pb(4)[:, 0:128], [P[4]], [v_])
                        dma("gpsimd", VV[r0:r0 + 128, :], v_.t[:], reads=[v_])

        def attn_swa(l):
            o_ = l // 2
            scale = 64 ** -0.5
            with Stage(k) as st:
                kT = st.sb("kT", [64, 2, T], BF16)
                vall = st.sb("vall", [128, 64, 128], BF16)
                for c0 in range(0, NTR * 128, 2048):
                    c1 = min(NTR * 128, c0 + 2048)
                    for kv in range(2):
                        dma("sync", kT.t[:, kv, c0:c1], KT[kv, :, c0:c1], writes=[kT])
                    dma("sync", vall.t[:, c0 // 128:c1 // 128, :], VV[c0:c1, :].rearrange("(j p) e -> p j e", p=128), writes=[vall])
                biasC = st.sb("biasC", [128, 2, 16, 128], BF16)
                for d in range(2):
                    dma("sync", biasC.t[:, d, :, :], Bscr[12:28, d, :, :].rearrange("h s q -> s h q"), writes=[biasC])
                op("vector", lambda e: e.memset(biasC.t[0:64, 1, :, 64:128], NEG), [], [biasC])
                sk = st.sb("sk", [1, 16], F32)
                es16 = st.sb("es16", [1, 16], F32)
                esrow = st.sb("esrow", [1, 16, 128], F32)
                dma("sync", sk.t[:], od_sinks[o_:o_ + 1, :], writes=[sk])
                op("scalar", lambda e: e.activation(out=es16.t[:], in_=sk.t[:], func=AF.Exp), [sk], [es16])
                for hh in range(16):
                    op("vector", lambda e: e.tensor_scalar(out=esrow.t[0:1, hh, :], in0=onesf.t[0:1, :], scalar1=es16.t[0:1, hh:hh + 1], scalar2=None, op0=ALU.mult), [onesf, es16], [esrow])
                qT = [st.sb(f"qT{i}", [64, 8, 128], BF16) for i in range(2)]
                PT = [st.sb(f"PT{i}", [128, 1024], BF16) for i in range(2)]
                rz = st.sb("rz", [64, 1024], F32)
                osb = [st.sb(f"osb{i}", [64, 8, 128], BF16) for i in range(2)]
                it = 0
                for qi in range(NTR):
                    tok0 = qi * 128
                    for kv in range(2):
                        q_ = qT[it % 2]
                        dma("sync", q_.t[:], QT[kv * 8:(kv + 1) * 8, :, tok0:tok0 + 128].rearrange("g d t -> d g t"), writes=[q_])
                        blks = ([(qi - 1, 1)] if qi >= 1 else []) + [(qi, 0)]
                        for bi, (j, d) in enumerate(blks):
                            fns = []
                            for half in range(2):
                                pk = 2 * (bi % 2) + half
                                fns.append(lambda e, pk=pk, half=half: e.matmul(k.pb(pk), lhsT=kT.t[:, kv, j * 128:(j + 1) * 128], rhs=q_.t[:, half * 4:(half + 1) * 4, :], start=True, stop=False))
                                fns.append(lambda e, pk=pk, half=half: e.matmul(k.pb(pk), lhsT=identb.t[:], rhs=biasC.t[:, d, kv * 8 + half * 4:kv * 8 + half * 4 + 4, :], start=False, stop=True))
                            pp_i = bi % 2
                            pe(fns, [kT, q_, identb, biasC], [P[2 * pp_i], P[2 * pp_i + 1]])
                            pt = PT[bi % 2]
                            op("scalar", lambda e: e.activation(out=pt.t[:], in_=k.pb2(pp_i), func=AF.Exp, scale=scale), [P[2 * pp_i], P[2 * pp_i + 1]], [pt])
                            fns = []
                            last = (bi == len(blks) - 1)
                            for half in range(2):
                                fns.append(lambda e, half=half: e.matmul(k.pb(4 + half)[0:64, :], lhsT=vall.t[:, j, kv * 64:(kv + 1) * 64], rhs=pt.t[:, half * 512:(half + 1) * 512], start=(bi == 0), stop=last))
                                fns.append(lambda e, half=half: e.matmul(k.pb(6 + half)[0:64, :], lhsT=onesb.t[:, 0:64], rhs=pt.t[:, half * 512:(half + 1) * 512], start=(bi == 0), stop=False))
                                if last:
                                    fns.append(lambda e, half=half: e.matmul(k.pb(6 + half)[0:64, :], lhsT=onesf.t[0:1, 0:64], rhs=esrow.t[0:1, kv * 8 + half * 4:kv * 8 + half * 4 + 4, :], start=False, stop=True))
                            pe(fns, [vall, pt, onesb, onesf, esrow], [P[4], P[5], P[6], P[7]])
                        op("vector", lambda e: e.reciprocal(out=rz.t[:], in_=k.pb2(3)[0:64, :]), [P[6], P[7]], [rz])
                        o_sb = osb[it % 2]
                        op("vector", lambda e: e.tensor_tensor(out=o_sb.t[:].rearrange("p a b -> p (a b)"), in0=k.pb2(2)[0:64, :], in1=rz.t[:], op=ALU.mult), [P[4], P[5], rz], [o_sb])
                        dma("gpsimd", OT16[kv * 8:(kv + 1) * 8, :, tok0:tok0 + 128].rearrange("g d t -> d g t"), o_sb.t[:], reads=[o_sb])
                        it += 1

        def tail(l):
            even = (l % 2 == 0)
            KP, nblk = (128, 8) if even else (64, 16)
            wsrc = ev_w_out_b[l // 2] if even else od_w_out_b[l // 2]
            OT = OT8 if even else OT16
            xscale = 64 ** -0.5
            tsub = stop[5] if (stop is not None and stop.startswith(f"tail{l}") and len(stop) == 6) else None
            with Stage(k) as st:
                wout = st.sb("wout", [KP, nblk, D], BF16)
                for c in range(4):
                    dma("sync", wout.t[:, c * (nblk // 4):(c + 1) * (nblk // 4), :], wsrc[c * 256:(c + 1) * 256, :].rearrange("(b p) n -> p b n", p=KP), writes=[wout])
                wq = st.sb("wq", [128, 8, 256], BF16)
                dma("sync", wq.t[:], wq_b[l].rearrange("(kc p) n -> p kc n", p=128), writes=[wq])
                wo = st.sb("wo", [64, 4, D], BF16)
                dma("sync", wo.t[:], wo_b[l].rearrange("(h d) n -> d h n", d=64), writes=[wo])
                kx = st.sb("kx", [64, 4, 256], BF16)
                vx = st.sb("vx", [128, 2, 256], BF16)
                dma("sync", kx.t[:], KXT[l], writes=[kx])
                dma("sync", vx.t[:], VX[l].rearrange("(i p) n -> p i n", p=128), writes=[vx])
                gb = []
                for i in range(1, 6):
                    g_ = st.sb(f"g{i}", [128, D], F32)
                    dma("sync", g_.t[:], norm_g[l, i:i + 1, :].partition_broadcast(128), writes=[g_])
                    gb.append(g_)
                g1, g2, g3, g4, g5 = gb
                xt = [st.sb(f"xt{i}", [128, D], F32) for i in range(4)]
                ysb = [st.sb(f"ysb{i}", [128, D], F32) for i in range(4)]
                tmp = st.sb("tmp", [128, D], F32)
                hb = st.sb("hb", [128, D], BF16)
                junk = st.sb("junk", [128, D], BF16)
                hT = st.sb("hT", [128, 8, 512], BF16)
                oT = st.sb("oT", [KP, nblk, 512], BF16)
                uT = st.sb("uT", [128, 32, 512], BF16)
                rr_ = st.sb("rr_", [128, 512], BF16)
                ws = [st.sb(f"ws{i}", [128, 8, 512], BF16) for i in range(3)]
                ss = st.sb("ss", [128, 4], F32)
                rstd = st.sb("rstd", [128, 4], F32)
                sspart = st.sb("sspart", [128, 8], F32)
                qx = st.sb("qx", [64, 4, 512], BF16)
                PTx = [st.sb(f"PTx{i}", [128, 512], BF16) for i in range(2)]
                rzx = st.sb("rzx", [64, 512], F32)
                ox = st.sb("ox", [64, 4, 512], BF16)
                xsrc = x_in if l == 0 else out
                wsi = 0
                for g in range(NGR if tsub != "s" else 0):
                    tok0 = g * 512
                    for t_ in range(4):
                        dma("sync", xt[t_].t[:], xsrc[tok0 + t_ * 128:tok0 + (t_ + 1) * 128, :], writes=[xt[t_]])
                    dma("sync", oT.t[:], OT[:, :, tok0:tok0 + 512].rearrange("b p t -> p b t"), writes=[oT])
                    if tsub == "x":
                        for t_ in range(4):
                            dma("gpsimd", out[tok0 + t_ * 128:tok0 + (t_ + 1) * 128, :], xt[t_].t[:], reads=[xt[t_]])
                        continue
                    for half in range(2):
                        for t_ in range(4):
                            pk = t_
                            pe([(lambda e, b=b: e.matmul(k.pb(pk), lhsT=oT.t[:, b, t_ * 128:(t_ + 1) * 128], rhs=wout.t[:, b, half * 512:(half + 1) * 512], start=(b == 0), stop=(b == nblk - 1))) for b in range(nblk)], [oT, wout], [P[pk]])
                            evac_y(pk, ysb[t_], half, sspart, t_, junk)
                    if tsub == "m":
                        for t_ in range(4):
                            dma("gpsimd", out[tok0 + t_ * 128:tok0 + (t_ + 1) * 128, :], ysb[t_].t[:], reads=[ysb[t_]])
                        continue
                    post_norm_add(st, xt, ysb, sspart, g1, rstd, tmp)
                    if tsub == "a":
                        for t_ in range(4):
                            dma("gpsimd", out[tok0 + t_ * 128:tok0 + (t_ + 1) * 128, :], xt[t_].t[:], reads=[xt[t_]])
                        continue
                    norm_to_hT(st, xt, g2, hb, hT, junk, ss, rstd, [4, 5])
                    for h in range(4):
                        pk = 6 + h % 2
                        pe([(lambda e, kc=kc: e.matmul(k.pb(pk)[0:64, :], lhsT=wq.t[:, kc, h * 64:(h + 1) * 64], rhs=hT.t[:, kc, :], start=(kc == 0), stop=(kc == 7))) for kc in range(8)], [wq, hT], [P[pk]])
                        k.copy(k.evac_eng(), qx.t[:, h, :], k.pb(pk)[0:64, :], [P[pk]], [qx])
                    for h in range(4):
                        for mb_ in range(2):
                            pk = mb_
                            pe([lambda e: e.matmul(k.pb(pk), lhsT=kx.t[:, h, mb_ * 128:(mb_ + 1) * 128], rhs=qx.t[:, h, :], start=True, stop=True)], [kx, qx], [P[pk]])
                            pt = PTx[mb_]
                            op("scalar", lambda e: e.activation(out=pt.t[:], in_=k.pb(pk), func=AF.Exp, scale=xscale), [P[pk]], [pt])
                            pe([lambda e: e.matmul(k.pb(2)[0:64, :], lhsT=vx.t[:, mb_, h * 64:(h + 1) * 64], rhs=pt.t[:], start=(mb_ == 0), stop=(mb_ == 1)),
                                lambda e: e.matmul(k.pb(3)[0:64, :], lhsT=onesb.t[:, 0:64], rhs=pt.t[:], start=(mb_ == 0), stop=(mb_ == 1))], [vx, pt, onesb], [P[2], P[3]])
                        op("vector", lambda e: e.reciprocal(out=rzx.t[:], in_=k.pb(3)[0:64, :]), [P[3]], [rzx])
                        op("vector", lambda e: e.tensor_tensor(out=ox.t[:, h, :], in0=k.pb(2)[0:64, :], in1=rzx.t[:], op=ALU.mult), [P[2], rzx], [ox])
                    for half in range(2):
                        for t_ in range(4):
                            pk = 4 + t_
                            pe([(lambda e, h=h: e.matmul(k.pb(pk), lhsT=ox.t[:, h, t_ * 128:(t_ + 1) * 128], rhs=wo.t[:, h, half * 512:(half + 1) * 512], start=(h == 0), stop=(h == 3))) for h in range(4)], [ox, wo], [P[pk]])
                            evac_y(pk, ysb[t_], half, sspart, t_, junk)
                    post_norm_add(st, xt, ysb, sspart, g3, rstd, tmp)
                    if tsub == "b":
                        for t_ in range(4):
                            dma("gpsimd", out[tok0 + t_ * 128:tok0 + (t_ + 1) * 128, :], xt[t_].t[:], reads=[xt[t_]])
                        continue
                    norm_to_hT(st, xt, g4, hb, hT, junk, ss, rstd, [0, 1])
                    for fb in range(8):
                        w_ = ws[wsi % 3]
                        wsi += 1
                        dma("sync", w_.t[:], w1_b[l][:, fb * 512:(fb + 1) * 512].rearrange("(kc p) n -> p kc n", p=128), writes=[w_])
                        for sub in range(4):
                            pk = 2 + sub % 2
                            pe([(lambda e, kc=kc: e.matmul(k.pb(pk), lhsT=w_.t[:, kc, sub * 128:(sub + 1) * 128], rhs=hT.t[:, kc, :], start=(kc == 0), stop=(kc == 7))) for kc in range(8)], [w_, hT], [P[pk]])
                            op("scalar", lambda e: e.activation(out=rr_.t[:], in_=k.pb(pk), func=AF.Relu), [P[pk]], [rr_])
                            op("vector", lambda e: e.tensor_tensor(out=uT.t[:, fb * 4 + sub, :], in0=rr_.t[:], in1=rr_.t[:], op=ALU.mult), [rr_], [uT])
                    for half in range(2):
                        for fq in range(4):
                            w_ = ws[wsi % 3]
                            wsi += 1
                            dma("sync", w_.t[:], w2_b[l][fq * 1024:(fq + 1) * 1024, half * 512:(half + 1) * 512].rearrange("(fc p) n -> p fc n", p=128), writes=[w_])
                            for t_ in range(4):
                                pk = 4 + t_
                                pe([(lambda e, fc=fc: e.matmul(k.pb(pk), lhsT=uT.t[:, fq * 8 + fc, t_ * 128:(t_ + 1) * 128], rhs=w_.t[:, fc, :], start=(fq == 0 and fc == 0), stop=(fq == 3 and fc == 7))) for fc in range(8)], [uT, w_], [P[pk]])
                        for t_ in range(4):
                            evac_y(4 + t_, ysb[t_], half, sspart, t_, junk)
                    post_norm_add(st, xt, ysb, sspart, g5, rstd, tmp)
                    for t_ in range(4):
                        dma("gpsimd", out[tok0 + t_ * 128:tok0 + (t_ + 1) * 128, :], xt[t_].t[:], reads=[xt[t_]])

        seq = []
        for l in range(DEPTH):
            if l % 2 == 0:
                seq += [(f"proj{l}", proj_even), (f"dsa{l}", attn_dsa), (f"diff{l}", attn_diff), (f"tail{l}", tail)]
            else:
                seq += [(f"proj{l}", proj_odd), (f"swa{l}", attn_swa), (f"tail{l}", tail)]
        if stop != "pre":
            for name, fn in seq:
                fn(int(name[-1]))
                if done(name) or (stop is not None and stop[:5] == name and name.startswith("tail")):
                    break
        k.barrier()
    return nc


_NAMES = ["rel_bias_table", "norm_g", "ev_w_in", "ev_a_kv_norm", "ev_a_w_uk", "ev_a_w_uv", "ev_b_lambda", "ev_b_subln",
          "ev_w_out", "od_w_in", "od_sinks", "od_w_out", "xa_wq", "xa_wkv", "xa_wo", "xa_mem_norm", "mlp_w1", "mlp_w2"]


def make_in_maps(inputs):
    oh, c128, hmask = host_consts()
    shared = {n: np.ascontiguousarray(np.asarray(inputs[n], dtype=np.float32)) for n in _NAMES}
    x = np.asarray(inputs["x"], dtype=np.float32)
    mem = np.asarray(inputs["mem"], dtype=np.float32)
    in_maps = []
    for c in range(8):
        b = c % 4
        m = dict(shared)
        m["x"] = np.ascontiguousarray(x[b])
        m["mem"] = np.ascontiguousarray(mem[b])
        m["c_oh"] = oh
        m["c_128"] = c128
        m["c_hmask"] = hmask
        in_maps.append(m)
    return in_maps


def kernel(**inputs):
    nc = build()
    in_maps = make_in_maps(inputs)
    res = run_bass_kernel_spmd(nc, in_maps[:4], core_ids=list(range(4)))
    return np.stack([np.asarray(res.results[b]["out"], dtype=np.float32) for b in range(4)], axis=0)
```

```python
import math
from contextlib import ExitStack
import numpy as np
import concourse.bass as bass
import concourse.mybir as mybir
from concourse.bass_utils import run_bass_kernel_spmd

F32 = mybir.dt.float32
BF16 = mybir.dt.bfloat16
AF = mybir.ActivationFunctionType
ALU = mybir.AluOpType
AX = mybir.AxisListType

T = 8192
NT = 64
NG = 16
D = 1024
EPS = 1e-6
NEG = -30000.0
DEPTH = 4
EV_COLS = 2760
OD_COLS = 1280
SAME_SYNC = True
NBIS = 16


class Sem:
    def __init__(self, h):
        self.h = h
        self.count = 0


class Buf:
    def __init__(self, name, t=None):
        self.name = name
        self.t = t
        self.w = None
        self.rs = {}
        self.dsem = None
        self.excl = False


class Eng:
    def __init__(self, name, obj, sem):
        self.name = name
        self.obj = obj
        self.sem = sem
        self.known = {}
        self.issued = {}


class Stage:
    def __init__(self, k):
        self.k = k
        self.es = ExitStack()
        self.bufs = []

    def sb(self, name, shape, dt):
        self.k.uid += 1
        t = self.es.enter_context(self.k.nc.sbuf_tensor(f"{name}_{self.k.uid}", shape, dt))
        b = Buf(name, t)
        self.bufs.append(b)
        return b

    def __enter__(self):
        return self

    def __exit__(self, *a):
        self.k.barrier()
        for b in self.bufs:
            if b.dsem is not None:
                for qn_, s_ in b.dsem.items():
                    self.k.free_dsems[qn_].append(s_)
                b.dsem = None
        self.es.close()
        return False


class K:
    def __init__(self, nc, es):
        self.nc = nc
        self.uid = 0
        self.E = {}
        for name in ("tensor", "vector", "scalar", "gpsimd", "sync"):
            s = Sem(es.enter_context(nc.semaphore(f"e_{name}")))
            self.E[name] = Eng(name, getattr(nc, name), s)
        self.free_dsems = {"sync": [], "gpsimd": []}
        for i in range(44):
            try:
                self.free_dsems["sync" if i % 2 == 0 else "gpsimd"].append(Sem(es.enter_context(nc.semaphore(f"d{i}"))))
            except KeyError:
                break
        self.pp = []
        self.P = []
        for i in range(4):
            t = es.enter_context(nc.psum_tensor(f"pp{i}", [128, 1024], F32))
            self.pp.append(t)
            self.P.append(Buf(f"P{2*i}"))
            self.P.append(Buf(f"P{2*i+1}"))
            self.P[-1].excl = True
            self.P[-2].excl = True
        self.rr = 0

    def pb(self, k):
        return self.pp[k // 2][:, (k % 2) * 512:(k % 2 + 1) * 512]

    def pb2(self, i):
        return self.pp[i][:, :]

    def pbb(self, k):
        return self.pp[k // 2][:, (k % 2) * 512:(k % 2 + 1) * 512].bitcast(BF16)

    def _deps(self, reads, writes):
        toks = {}
        writes = list(writes) + [b for b in reads if b.excl]

        def add(sem, v):
            if toks.get(sem, 0) < v:
                toks[sem] = v
        for b in reads:
            if b.w is not None:
                add(*b.w)
        for b in writes:
            if b.w is not None:
                add(*b.w)
            for sem, v in b.rs.items():
                add(sem, v)
        return toks

    def _wait(self, eng, toks, skip=None):
        for sem, v in toks.items():
            if sem is skip:
                continue
            if sem is eng.sem and (eng.name == "tensor" or not SAME_SYNC):
                continue
            if eng.known.get(sem, 0) >= v:
                continue
            eng.obj.wait_ge(sem.h, v)
            eng.known[sem] = v

    def _mark(self, tok, reads, writes):
        writes = list(writes) + [b for b in reads if b.excl]
        reads = [b for b in reads if not b.excl]
        for b in reads:
            if b.rs.get(tok[0], 0) < tok[1]:
                b.rs[tok[0]] = tok[1]
        for b in writes:
            b.w = tok
            b.rs = {}

    def op(self, engname, fn, reads=(), writes=()):
        eng = self.E[engname]
        self._wait(eng, self._deps(reads, writes))
        ins = fn(eng.obj)
        eng.sem.count += 1
        ins.then_inc(eng.sem.h, 1)
        self._mark((eng.sem, eng.sem.count), reads, writes)
        return ins

    def pe(self, fns, reads=(), writes=()):
        eng = self.E["tensor"]
        self._wait(eng, self._deps(reads, writes))
        ins = None
        for f in fns:
            ins = f(eng.obj)
        eng.sem.count += 1
        ins.then_inc(eng.sem.h, 1)
        self._mark((eng.sem, eng.sem.count), reads, writes)

    def dma(self, qname, out, in_, reads=(), writes=(), sembuf=None, **kw):
        q = self.E[qname]
        b = sembuf or (writes[0] if writes else reads[0])
        if b.dsem is None:
            b.dsem = {}
        if qname not in b.dsem:
            b.dsem[qname] = self.free_dsems[qname].pop()
        ds = b.dsem[qname]
        self._wait(q, self._deps(reads, writes), skip=ds)
        ins = q.obj.dma_start(out=out, in_=in_, **kw)
        ds.count += 16
        ins.then_inc(ds.h, 16)
        q.issued[ds] = ds.count
        self._mark((ds, ds.count), reads, writes)

    def barrier(self):
        for qn in ("sync", "gpsimd", "scalar", "vector"):
            q = self.E[qn]
            for sem, v in list(q.issued.items()):
                if q.known.get(sem, 0) < v:
                    q.obj.wait_ge(sem.h, v)
                    q.known[sem] = v
            q.issued = {}
        q = self.E["sync"]
        q.obj.sem_inc(q.sem.h, 1)
        q.sem.count += 1
        g = self.E["gpsimd"]
        g.obj.sem_inc(g.sem.h, 1)
        g.sem.count += 1
        for e in self.E.values():
            for o in self.E.values():
                if o is e:
                    continue
                if e.known.get(o.sem, 0) < o.sem.count:
                    e.obj.wait_ge(o.sem.h, o.sem.count)
                    e.known[o.sem] = o.sem.count
        for p in self.P:
            p.w = None
            p.rs = {}

    def evac_eng(self):
        self.rr += 1
        return "vector" if self.rr % 2 else "scalar"

    def copy(self, engname, out, in_, reads, writes):
        if engname == "scalar":
            return self.op("scalar", lambda e: e.copy(out=out, in_=in_), reads, writes)
        return self.op(engname, lambda e: e.tensor_copy(out=out, in_=in_), reads, writes)


def t5_bucket_np(rel):
    nb = 16
    max_exact = 8
    n = np.abs(rel)
    nf = np.maximum(n, 1).astype(np.float32)
    large = max_exact + (np.log(nf / max_exact) / math.log(1024 / max_exact) * (nb - max_exact)).astype(np.int32)
    large = np.minimum(large, nb - 1)
    return np.where(rel > 0, nb, 0) + np.where(n < max_exact, n, large)


def host_consts():
    s = np.arange(128)[:, None]
    q = np.arange(128)[None, :]
    oh = np.zeros((32, 6, 128, 128), np.float32)
    for d in range(6):
        rel = (s - q) - 128 * d
        b = t5_bucket_np(rel)
        for bb in range(32):
            oh[bb, d] = (b == bb)
    oh = oh.reshape(32, 6 * 16384)
    c128 = np.zeros((128, 16 + 128 + 128), np.float32)
    c128[:, 0:16] = (2.0 ** -np.arange(16))[None, :]
    qq = np.arange(128)[:, None]
    ss = np.arange(128)[None, :]
    c128[:, 16:144] = np.where((qq < 64) & (ss >= 64), NEG, 0.0)
    c128[:, 144:272] = np.eye(128, dtype=np.float32)
    hmask = np.zeros((28, 1), np.float32)
    hmask[:12] = 1.0
    return oh, c128, hmask


def build(stop=None, debug=(), lim=16):
    NGR = lim
    NTR = 4 * lim
    nc = bass.Bass("TRN2", target_bir_lowering=False)

    def din(name, shape, dt=F32):
        return nc.dram_tensor(name, list(shape), dt, kind="ExternalInput").ap()

    def dtmp(name, shape, dt):
        kind = "ExternalOutput" if name in debug else "Internal"
        return nc.dram_tensor(name, list(shape), dt, kind=kind).ap()

    x_in = din("x", [T, D])
    mem_in = din("mem", [256, D])
    table = din("rel_bias_table", [32, 28])
    norm_g = din("norm_g", [4, 6, D])
    ev_w_in = din("ev_w_in", [2, D, EV_COLS])
    ev_a_kv_norm = din("ev_a_kv_norm", [2, 128])
    ev_a_w_uk = din("ev_a_w_uk", [2, 8, 64, 128])
    ev_a_w_uv = din("ev_a_w_uv", [2, 8, 128, 64])
    ev_b_lambda = din("ev_b_lambda", [2, 4, 64])
    ev_b_subln = din("ev_b_subln", [2, 128])
    ev_w_out = din("ev_w_out", [2, D, D])
    od_w_in = din("od_w_in", [2, D, OD_COLS])
    od_sinks = din("od_sinks", [2, 16])
    od_w_out = din("od_w_out", [2, D, D])
    xa_wq = din("xa_wq", [4, D, 256])
    xa_wkv = din("xa_wkv", [4, D, 512])
    xa_wo = din("xa_wo", [4, 256, D])
    xa_mem_norm = din("xa_mem_norm", [4, D])
    mlp_w1 = din("mlp_w1", [4, D, 4096])
    mlp_w2 = din("mlp_w2", [4, 4096, D])
    oh_in = din("c_oh", [32, 6 * 16384])
    c128_in = din("c_128", [128, 272])
    hmask_in = din("c_hmask", [28, 1])
    out = nc.dram_tensor("out", [T, D], F32, kind="ExternalOutput").ap()

    ev_w_in_b = dtmp("ev_w_in_b", [2, D, EV_COLS], BF16)
    ev_w_out_b = dtmp("ev_w_out_b", [2, D, D], BF16)
    od_w_in_b = dtmp("od_w_in_b", [2, D, OD_COLS], BF16)
    od_w_out_b = dtmp("od_w_out_b", [2, D, D], BF16)
    wuk_b = dtmp("wuk_b", [2, 8, 64, 128], BF16)
    wuv_b = dtmp("wuv_b", [2, 8, 128, 64], BF16)
    wq_b = dtmp("wq_b", [4, D, 256], BF16)
    wkv_b = dtmp("wkv_b", [4, D, 512], BF16)
    wo_b = dtmp("wo_b", [4, 256, D], BF16)
    w1_b = dtmp("w1_b", [4, D, 4096], BF16)
    w2_b = dtmp("w2_b", [4, 4096, D], BF16)
    Bscr = dtmp("Bscr", [28, 6, 128, 128], BF16)
    KXT = dtmp("KXT", [4, 64, 4, 256], BF16)
    VX = dtmp("VX", [4, 256, 256], BF16)
    QLT = dtmp("QLT", [8, 128, T], BF16)
    QIT = dtmp("QIT", [8, 64, T], BF16)
    WIX = dtmp("WIX", [T, 8], F32)
    QBT = dtmp("QBT", [8, 64, T], BF16)
    CKV = dtmp("CKV", [T, 128], BF16)
    CKVT = dtmp("CKVT", [128, T], BF16)
    KIT = dtmp("KIT", [64, T], BF16)
    KBT = dtmp("KBT", [8, 64, T], BF16)
    VB = dtmp("VB", [T, 512], BF16)
    OT8 = dtmp("OT8", [8, 128, T], BF16)
    QT = dtmp("QT", [16, 64, T], BF16)
    KT = dtmp("KT", [2, 64, T], BF16)
    VV = dtmp("VV", [T, 128], BF16)
    OT16 = dtmp("OT16", [16, 64, T], BF16)

    with ExitStack() as es:
        k = K(nc, es)
        op, pe, dma = k.op, k.pe, k.dma
        P = k.P

        def gsb(name, shape, dt):
            t = es.enter_context(nc.sbuf_tensor(name, shape, dt))
            return Buf(name, t)

        identf = gsb("identf", [128, 128], F32)
        identb = gsb("identb", [128, 128], BF16)
        onesb = gsb("onesb", [128, 128], BF16)
        onesf = gsb("onesf", [128, 128], F32)
        epsb = gsb("epsb", [128, 1], F32)
        cneg = gsb("cneg", [128, 128], F32)
        pow2 = gsb("pow2", [128, 16], F32)
        neglam = gsb("neglam", [128, 2], F32)
        gsub = gsb("gsub", [128, 2], F32)

        def done(name):
            return stop == name

        with Stage(k) as st:
            dma("sync", pow2.t[:], c128_in[:, 0:16], writes=[pow2])
            dma("sync", cneg.t[:], c128_in[:, 16:144], writes=[cneg])
            dma("sync", identf.t[:], c128_in[:, 144:272], writes=[identf])
            op("vector", lambda e: e.tensor_copy(out=identb.t[:], in_=identf.t[:]), [identf], [identb])
            op("vector", lambda e: e.memset(onesb.t[:], 1.0), [], [onesb])
            op("vector", lambda e: e.memset(onesf.t[:], 1.0), [], [onesf])
            op("vector", lambda e: e.memset(epsb.t[:], EPS), [], [epsb])
            castb = Buf("castb")
            if stop == "s0a":
                k.barrier()
                return nc

            def cast2d(dst, src, rows):
                for r0 in range(0, rows, 256):
                    r1 = min(rows, r0 + 256)
                    dma("gpsimd", dst[r0:r1, :], src[r0:r1, :], sembuf=castb)
            for e_ in range(2):
                cast2d(ev_w_in_b[e_], ev_w_in[e_], D)
                cast2d(ev_w_out_b[e_], ev_w_out[e_], D)
                cast2d(od_w_in_b[e_], od_w_in[e_], D)
                cast2d(od_w_out_b[e_], od_w_out[e_], D)
                cast2d(wuk_b[e_].rearrange("h d l -> (h d) l"), ev_a_w_uk[e_].rearrange("h d l -> (h d) l"), 512)
                cast2d(wuv_b[e_].rearrange("h l d -> (h l) d"), ev_a_w_uv[e_].rearrange("h l d -> (h l) d"), 1024)
            for l in range(4):
                cast2d(wq_b[l], xa_wq[l], D)
                cast2d(wkv_b[l], xa_wkv[l], D)
                cast2d(wo_b[l], xa_wo[l], 256)
                cast2d(w1_b[l], mlp_w1[l], D)
                cast2d(w2_b[l], mlp_w2[l], 4096)
            lrow = st.sb("lrow", [1, 2, 256], F32)
            prod = st.sb("prod", [1, 2, 2, 64], F32)
            ssum = st.sb("ssum", [1, 4], F32)
            esum = st.sb("esum", [1, 4], F32)
            nlr = st.sb("nlr", [1, 2], F32)
            sub_t = st.sb("sub_t", [128, 2], F32)
            for e_ in range(2):
                dma("sync", lrow.t[0:1, e_, :], ev_b_lambda[e_:e_ + 1, :, :].rearrange("o a b -> o (a b)"), writes=[lrow])
                dma("sync", sub_t.t[:, e_:e_ + 1], ev_b_subln[e_:e_ + 1, :].rearrange("o p -> p o"), writes=[sub_t])
            for e_ in range(2):
                lam_init = 0.8 - 0.6 * math.exp(-0.3 * (2 * e_))
                for j in range(2):
                    op("vector", lambda e: e.tensor_tensor(out=prod.t[0:1, e_, j, :], in0=lrow.t[0:1, e_, (2 * j) * 64:(2 * j + 1) * 64],
                                                           in1=lrow.t[0:1, e_, (2 * j + 1) * 64:(2 * j + 2) * 64], op=ALU.mult), [lrow], [prod])
                    op("vector", lambda e: e.tensor_reduce(out=ssum.t[0:1, 2 * e_ + j:2 * e_ + j + 1], in_=prod.t[0:1, e_, j, :], axis=AX.X, op=ALU.add), [prod], [ssum])
                op("scalar", lambda e: e.activation(out=esum.t[0:1, 2 * e_:2 * e_ + 2], in_=ssum.t[0:1, 2 * e_:2 * e_ + 2], func=AF.Exp), [ssum], [esum])
                op("vector", lambda e: e.tensor_tensor(out=nlr.t[0:1, e_:e_ + 1], in0=esum.t[0:1, 2 * e_ + 1:2 * e_ + 2], in1=esum.t[0:1, 2 * e_:2 * e_ + 1], op=ALU.subtract), [esum], [nlr])
                op("vector", lambda e: e.tensor_scalar(out=nlr.t[0:1, e_:e_ + 1], in0=nlr.t[0:1, e_:e_ + 1], scalar1=-lam_init, scalar2=None, op0=ALU.add), [nlr], [nlr])
                op("vector", lambda e: e.tensor_scalar(out=gsub.t[:, e_:e_ + 1], in0=sub_t.t[:, e_:e_ + 1], scalar1=(1.0 - lam_init), scalar2=None, op0=ALU.mult), [sub_t], [gsub])
            pe([lambda e: e.matmul(k.pb(0)[:, 0:2], lhsT=onesf.t[0:1, :], rhs=nlr.t[0:1, :], start=True, stop=True)], [onesf, nlr], [P[0]])
            op("vector", lambda e: e.tensor_copy(out=neglam.t[:], in_=k.pb(0)[:, 0:2]), [P[0]], [neglam])

        with Stage(k) as st:
            tab = st.sb("tab", [32, 28], F32)
            cm = st.sb("cm", [28, 1], F32)
            hm = st.sb("hm", [28, 1], F32)
            dma("sync", tab.t[:], table[:, :], writes=[tab])
            dma("sync", cm.t[:], table[15:16, :].rearrange("o h -> h o"), writes=[cm])
            dma("sync", hm.t[:], hmask_in[:, :], writes=[hm])
            op("vector", lambda e: e.tensor_tensor(out=cm.t[:], in0=cm.t[:], in1=hm.t[:], op=ALU.mult), [hm], [cm])
            ohb = [st.sb(f"ohb{i}", [32, 4096], F32) for i in range(2)]
            bsb = [st.sb(f"bsb{i}", [28, 4096], BF16) for i in range(2)]
            Bflat = Bscr.rearrange("h d s q -> h (d s q)")
            for i in range(24):
                o_ = ohb[i % 2]
                b_ = bsb[i % 2]
                dma("sync", o_.t[:], oh_in[:, i * 4096:(i + 1) * 4096], writes=[o_])
                for j in range(8):
                    pk = (i * 8 + j) % 4
                    pe([lambda e: e.matmul(k.pb(pk)[0:28, :], lhsT=tab.t[:, :], rhs=o_.t[:, j * 512:(j + 1) * 512], start=True, stop=True)], [tab, o_], [P[pk]])
                    op("vector", lambda e: e.tensor_scalar(out=b_.t[:, j * 512:(j + 1) * 512], in0=k.pb(pk)[0:28, :], scalar1=cm.t[:, 0:1], scalar2=8.0, op0=ALU.subtract, op1=ALU.mult), [P[pk], cm], [b_])
                dma("gpsimd", Bflat[:, i * 4096:(i + 1) * 4096], b_.t[:], reads=[b_])
        with Stage(k) as st:
            negt = st.sb("negt", [28, 64, 64], BF16)
            op("vector", lambda e: e.memset(negt.t[:], NEG), [], [negt])
            dma("gpsimd", Bscr[:, 0, 64:128, 0:64], negt.t[:], reads=[negt])

        with Stage(k) as st:
            mt = [st.sb(f"mt{i}", [128, D], F32) for i in range(2)]
            for i in range(2):
                dma("sync", mt[i].t[:], mem_in[i * 128:(i + 1) * 128, :], writes=[mt[i]])
            junk = st.sb("junk", [128, D], BF16)
            ss = st.sb("ss", [128, 2], F32)
            rstd = st.sb("rstd", [128, 2], F32)
            for i in range(2):
                op("scalar", lambda e: e.activation(out=junk.t[:], in_=mt[i].t[:], func=AF.Square, accum_out=ss.t[:, i:i + 1]), [mt[i]], [junk, ss])
            op("scalar", lambda e: e.activation(out=rstd.t[:], in_=ss.t[:], func=AF.Sqrt, bias=epsb.t[:, 0:1], scale=1.0 / D), [ss, epsb], [rstd])
            op("vector", lambda e: e.reciprocal(out=rstd.t[:], in_=rstd.t[:]), [rstd], [rstd])
            gm = st.sb("gm", [128, D], F32)
            mb = st.sb("mb", [128, D], BF16)
            memT = st.sb("memT", [128, 8, 256], BF16)
            wkv = st.sb("wkv", [128, 8, 512], BF16)
            kx = st.sb("kx", [64, 4, 256], BF16)
            vx = st.sb("vx", [128, 2, 256], BF16)
            for l in range(4):
                dma("sync", gm.t[:], xa_mem_norm[l:l + 1, :].partition_broadcast(128), writes=[gm])
                dma("sync", wkv.t[:], wkv_b[l].rearrange("(kc p) n -> p kc n", p=128), writes=[wkv])
                for i in range(2):
                    op("vector", lambda e: e.scalar_tensor_tensor(out=mb.t[:], in0=mt[i].t[:], scalar=rstd.t[:, i:i + 1], in1=gm.t[:], op0=ALU.mult, op1=ALU.mult), [mt[i], rstd, gm], [mb])
                    pe([(lambda e, kc=kc: e.transpose(out=k.pbb(0)[:, kc * 128:(kc + 1) * 128], in_=mb.t[:, kc * 128:(kc + 1) * 128], identity=identb.t[:])) for kc in range(8)], [mb, identb], [P[0]])
                    op("vector", lambda e: e.tensor_copy(out=memT.t[:, :, i * 128:(i + 1) * 128], in_=k.pbb(0).rearrange("p (a b) -> p a b", a=8)), [P[0]], [memT])
                for h in range(4):
                    pe([(lambda e, kc=kc: e.matmul(k.pb(1)[0:64, 0:256], lhsT=wkv.t[:, kc, h * 64:(h + 1) * 64], rhs=memT.t[:, kc, :], start=(kc == 0), stop=(kc == 7))) for kc in range(8)], [wkv, memT], [P[1]])
                    op("vector", lambda e: e.tensor_copy(out=kx.t[:, h, :], in_=k.pb(1)[0:64, 0:256]), [P[1]], [kx])
                for i in range(2):
                    pe([(lambda e, kc=kc: e.matmul(k.pb(2)[:, 0:256], lhsT=memT.t[:, kc, i * 128:(i + 1) * 128], rhs=wkv.t[:, kc, 256:512], start=(kc == 0), stop=(kc == 7))) for kc in range(8)], [wkv, memT], [P[2]])
                    op("vector", lambda e: e.tensor_copy(out=vx.t[:, i, :], in_=k.pb(2)[:, 0:256]), [P[2]], [vx])
                dma("gpsimd", KXT[l], kx.t[:], reads=[kx])
                dma("gpsimd", VX[l].rearrange("(i p) n -> p i n", p=128), vx.t[:], reads=[vx])

        def norm_to_hT(st, xt_list, gbc, hb, hT, junk, ss, rstd, pbank):
            n = len(xt_list)
            for t_ in range(n):
                op("scalar", lambda e: e.activation(out=junk.t[:], in_=xt_list[t_].t[:], func=AF.Square, accum_out=ss.t[:, t_:t_ + 1]), [xt_list[t_]], [junk, ss])
            op("scalar", lambda e: e.activation(out=rstd.t[:, 0:n], in_=ss.t[:, 0:n], func=AF.Sqrt, bias=epsb.t[:, 0:1], scale=1.0 / D), [ss, epsb], [rstd])
            op("vector", lambda e: e.reciprocal(out=rstd.t[:, 0:n], in_=rstd.t[:, 0:n]), [rstd], [rstd])
            for t_ in range(n):
                op("vector", lambda e: e.scalar_tensor_tensor(out=hb.t[:], in0=xt_list[t_].t[:], scalar=rstd.t[:, t_:t_ + 1], in1=gbc.t[:], op0=ALU.mult, op1=ALU.mult), [xt_list[t_], rstd, gbc], [hb])
                pk = pbank[t_ % len(pbank)]
                pe([(lambda e, kc=kc: e.transpose(out=k.pbb(pk)[:, kc * 128:(kc + 1) * 128], in_=hb.t[:, kc * 128:(kc + 1) * 128], identity=identb.t[:])) for kc in range(8)], [hb, identb], [P[pk]])
                k.copy(k.evac_eng(), hT.t[:, :, t_ * 128:(t_ + 1) * 128], k.pbb(pk).rearrange("p (a b) -> p a b", a=8), [P[pk]], [hT])

        pn_a = gsb("pn_a", [128, 4], F32)
        pn_b = gsb("pn_b", [128, 4], F32)

        def post_norm_add(st, xt_list, ysb_list, sspart, gbc, rstd, tmp):
            n = len(xt_list)
            ssv = sspart.t[:, 0:2 * n].rearrange("p (t h) -> p t h", h=2)
            op("vector", lambda e: e.tensor_reduce(out=pn_a.t[:, 0:n], in_=ssv, axis=AX.X, op=ALU.add), [sspart], [pn_a])
            op("scalar", lambda e: e.activation(out=pn_b.t[:, 0:n], in_=pn_a.t[:, 0:n], func=AF.Sqrt, bias=epsb.t[:, 0:1], scale=1.0 / D), [pn_a, epsb], [pn_b])
            op("vector", lambda e: e.reciprocal(out=rstd.t[:, 0:n], in_=pn_b.t[:, 0:n]), [pn_b], [rstd])
            for t_ in range(n):
                op("vector", lambda e: e.scalar_tensor_tensor(out=tmp.t[:], in0=ysb_list[t_].t[:], scalar=rstd.t[:, t_:t_ + 1], in1=gbc.t[:], op0=ALU.mult, op1=ALU.mult), [ysb_list[t_], rstd, gbc], [tmp])
                op("vector", lambda e: e.tensor_tensor(out=xt_list[t_].t[:], in0=xt_list[t_].t[:], in1=tmp.t[:], op=ALU.add), [tmp], [xt_list[t_]])

        def evac_y(pk, ysb, half, sspart, t_, junk):
            op("vector", lambda e: e.tensor_copy(out=ysb.t[:, half * 512:(half + 1) * 512], in_=k.pb(pk)), [P[pk]], [ysb])
            op("scalar", lambda e: e.activation(out=junk.t[:, 0:512], in_=k.pb(pk), func=AF.Square, accum_out=sspart.t[:, 2 * t_ + half:2 * t_ + half + 1]), [P[pk]], [junk, sspart])

        def proj_even(l):
            e_ = l // 2
            xsrc = x_in if l == 0 else out
            with Stage(k) as st:
                win = st.sb("win", [128, 8, EV_COLS], BF16)
                for kc in range(8):
                    dma("sync", win.t[:, kc, :], ev_w_in_b[e_][kc * 128:(kc + 1) * 128, :], writes=[win])
                wuk = st.sb("wuk", [128, 4, 128], BF16)
                dma("sync", wuk.t[:], wuk_b[e_].rearrange("(hp par) d l -> (par d) hp l", par=2), writes=[wuk])
                g0 = st.sb("g0", [128, D], F32)
                dma("sync", g0.t[:], norm_g[l, 0:1, :].partition_broadcast(128), writes=[g0])
                akv = st.sb("akv", [128, 128], F32)
                dma("sync", akv.t[:], ev_a_kv_norm[e_:e_ + 1, :].partition_broadcast(128), writes=[akv])
                xt = [st.sb(f"xt{i}", [128, D], F32) for i in range(4)]
                hb = st.sb("hb", [128, D], BF16)
                junk = st.sb("junk", [128, D], BF16)
                hT = st.sb("hT", [128, 8, 512], BF16)
                ss = st.sb("ss", [128, 4], F32)
                rstd = st.sb("rstd", [128, 4], F32)
                qaT = st.sb("qaT", [128, 4, 512], BF16)
                fm = [st.sb(f"fm{i}", [128, 512], BF16) for i in range(3)]
                ssc = st.sb("ssc", [128, 1], F32)
                rsc = st.sb("rsc", [128, 1], F32)
                ckv_sb = [st.sb(f"ckv_sb{i}", [128, 128], BF16) for i in range(2)]
                ckvT_sb = [st.sb(f"ckvT_sb{i}", [128, 128], BF16) for i in range(2)]
                wi_sb = [st.sb(f"wi_sb{i}", [128, 8], F32) for i in range(2)]
                vb_sb = [st.sb(f"vb_sb{i}", [128, 512], BF16) for i in range(2)]
                fmi = 0
                for g in range(NGR):
                    tok0 = g * 512
                    for t_ in range(4):
                        dma("sync", xt[t_].t[:], xsrc[tok0 + t_ * 128:tok0 + (t_ + 1) * 128, :], writes=[xt[t_]])
                    norm_to_hT(st, xt, g0, hb, hT, junk, ss, rstd, [0, 1])
                    blocks = []
                    for p_ in range(4):
                        blocks.append((p_ * 128, 128, ("qa", p_)))
                    for p_ in range(4):
                        blocks.append((640 + p_ * 128, 128, ("st", QIT[2 * p_:2 * p_ + 2].rearrange("a d t -> (a d) t"))))
                    blocks.append((1152, 64, ("st", KIT)))
                    for p_ in range(4):
                        blocks.append((1224 + p_ * 128, 128, ("st", QBT[2 * p_:2 * p_ + 2].rearrange("a d t -> (a d) t"))))
                    for p_ in range(4):
                        blocks.append((1736 + p_ * 128, 128, ("st", KBT[2 * p_:2 * p_ + 2].rearrange("a d t -> (a d) t"))))
                    for bi, (c0, ncol, dest) in enumerate(blocks):
                        pk = 2 + bi % 2
                        pe([(lambda e, kc=kc: e.matmul(k.pb(pk)[0:ncol, :], lhsT=win.t[:, kc, c0:c0 + ncol], rhs=hT.t[:, kc, :], start=(kc == 0), stop=(kc == 7))) for kc in range(8)], [win, hT], [P[pk]])
                        if dest[0] == "qa":
                            k.copy(k.evac_eng(), qaT.t[:, dest[1], :], k.pb(pk), [P[pk]], [qaT])
                        else:
                            f_ = fm[fmi % 3]
                            fmi += 1
                            k.copy(k.evac_eng(), f_.t[0:ncol, :], k.pb(pk)[0:ncol, :], [P[pk]], [f_])
                            dma("gpsimd", dest[1][:, tok0:tok0 + 512], f_.t[0:ncol, :], reads=[f_])
                    for h in range(8):
                        pk = 2 + h % 2
                        b0 = (h % 2) * 64
                        pe([lambda e: e.matmul(k.pb(pk), lhsT=wuk.t[b0:b0 + 64, h // 2, :], rhs=qaT.t[b0:b0 + 64, h // 2, :], start=True, stop=True)], [wuk, qaT], [P[pk]])
                        f_ = fm[fmi % 3]
                        fmi += 1
                        k.copy(k.evac_eng(), f_.t[:], k.pb(pk), [P[pk]], [f_])
                        dma("gpsimd", QLT[h, :, tok0:tok0 + 512], f_.t[:], reads=[f_])
                    for t_ in range(4):
                        r0 = tok0 + t_ * 128
                        i2 = t_ % 2
                        pe([(lambda e, kc=kc: e.matmul(k.pb(4)[:, 0:128], lhsT=hT.t[:, kc, t_ * 128:(t_ + 1) * 128], rhs=win.t[:, kc, 512:640], start=(kc == 0), stop=(kc == 7))) for kc in range(8)]
                           + [(lambda e, kc=kc: e.matmul(k.pb(4)[:, 128:136], lhsT=hT.t[:, kc, t_ * 128:(t_ + 1) * 128], rhs=win.t[:, kc, 1216:1224], start=(kc == 0), stop=(kc == 7))) for kc in range(8)], [win, hT], [P[4]])
                        pe([(lambda e, kc=kc: e.matmul(k.pb(5), lhsT=hT.t[:, kc, t_ * 128:(t_ + 1) * 128], rhs=win.t[:, kc, 2248:2760], start=(kc == 0), stop=(kc == 7))) for kc in range(8)], [win, hT], [P[5]])
                        op("scalar", lambda e: e.activation(out=junk.t[:, 0:128], in_=k.pb(4)[:, 0:128], func=AF.Square, accum_out=ssc.t[:, 0:1]), [P[4]], [junk, ssc])
                        op("scalar", lambda e: e.activation(out=rsc.t[:], in_=ssc.t[:], func=AF.Sqrt, bias=epsb.t[:, 0:1], scale=1.0 / 128), [ssc, epsb], [rsc])
                        op("vector", lambda e: e.reciprocal(out=rsc.t[:], in_=rsc.t[:]), [rsc], [rsc])
                        op("vector", lambda e: e.scalar_tensor_tensor(out=ckv_sb[i2].t[:], in0=k.pb(4)[:, 0:128], scalar=rsc.t[:, 0:1], in1=akv.t[:], op0=ALU.mult, op1=ALU.mult), [P[4], rsc, akv], [ckv_sb[i2]])
                        op("vector", lambda e: e.tensor_copy(out=wi_sb[i2].t[:], in_=k.pb(4)[:, 128:136]), [P[4]], [wi_sb[i2]])
                        dma("gpsimd", CKV[r0:r0 + 128, :], ckv_sb[i2].t[:], reads=[ckv_sb[i2]])
                        dma("gpsimd", WIX[r0:r0 + 128, :], wi_sb[i2].t[:], reads=[wi_sb[i2]])
                        pe([lambda e: e.transpose(out=k.pbb(6)[:, 0:128], in_=ckv_sb[i2].t[:], identity=identb.t[:])], [ckv_sb[i2], identb], [P[6]])
                        op("vector", lambda e: e.tensor_copy(out=ckvT_sb[i2].t[:], in_=k.pbb(6)[:, 0:128]), [P[6]], [ckvT_sb[i2]])
                        dma("gpsimd", CKVT[:, r0:r0 + 128], ckvT_sb[i2].t[:], reads=[ckvT_sb[i2]])
                        op("scalar", lambda e: e.copy(out=vb_sb[i2].t[:], in_=k.pb(5)), [P[5]], [vb_sb[i2]])
                        dma("gpsimd", VB[r0:r0 + 128, :], vb_sb[i2].t[:], reads=[vb_sb[i2]])

        def attn_dsa(l):
            e_ = l // 2
            with Stage(k) as st:
                ckvT = st.sb("ckvT", [128, T], BF16)
                ckv = st.sb("ckv", [128, 64, 128], BF16)
                kiT = st.sb("kiT", [64, T], BF16)
                for c0 in range(0, NTR * 128, 2048):
                    c1 = min(NTR * 128, c0 + 2048)
                    dma("sync", ckvT.t[:, c0:c1], CKVT[:, c0:c1], writes=[ckvT])
                    dma("sync", kiT.t[:, c0:c1], KIT[:, c0:c1], writes=[kiT])
                    dma("sync", ckv.t[:, c0 // 128:c1 // 128, :], CKV[c0:c1, :].rearrange("(j p) l -> p j l", p=128), writes=[ckv])
                wuv = st.sb("wuv", [128, 8, 64], BF16)
                dma("sync", wuv.t[:], wuv_b[e_].rearrange("h l d -> l h d"), writes=[wuv])
                biasA = st.sb("biasA", [128, 6, 8, 128], BF16)
                for d in range(6):
                    dma("sync", biasA.t[:, d, :, :], Bscr[0:8, d, :, :].rearrange("h s q -> s h q"), writes=[biasA])
                sel8 = st.sb("sel8", [128, 8, 128], BF16)
                for h in range(8):
                    op("vector", lambda e: e.tensor_copy(out=sel8.t[:, h, :], in_=identb.t[:]), [identb], [sel8])
                I = st.sb("I", [128, T], F32)
                junk = st.sb("junk", [128, 2048], BF16)
                negm = [st.sb(f"negm{i}", [128, T], BF16) for i in range(2)]
                qiT = [st.sb(f"qiT{i}", [64, 8, 128], BF16) for i in range(2)]
                qlT = [st.sb(f"qlT{i}", [128, 8, 128], BF16) for i in range(2)]
                wi = [st.sb(f"wi{i}", [128, 8], F32) for i in range(2)]
                arel = [st.sb(f"arel{i}", [128, 512], BF16) for i in range(3)]
                amp = st.sb("amp", [128, 16], F32)
                amax = st.sb("amax", [128, 1], F32)
                lo = st.sb("lo", [128, 1], F32)
                mid = st.sb("mid", [128, 1], F32)
                cnt = st.sb("cnt", [128, 1], F32)
                tmp1 = st.sb("tmp1", [128, 1], F32)
                wall = st.sb("wall", [128, 16], F32)
                PT = [st.sb(f"PT{i}", [128, 1024], BF16) for i in range(2)]
                rz = st.sb("rz", [128, 1024], F32)
                oaT = st.sb("oaT", [128, 8, 128], BF16)
                oub = [st.sb(f"oub{i}", [128, 4, 128], BF16) for i in range(2)]
                scale = 64 ** -0.5
                aic = [0]

                def idx_phase(qi):
                    ai = aic[0]
                    tok0 = qi * 128
                    b2 = qi % 2
                    dma("sync", qiT[b2].t[:], QIT[:, :, tok0:tok0 + 128].rearrange("h d t -> d h t"), writes=[qiT[b2]])
                    dma("sync", qlT[b2].t[:], QLT[:, :, tok0:tok0 + 128].rearrange("h l t -> l h t"), writes=[qlT[b2]])
                    dma("sync", wi[b2].t[:], WIX[tok0:tok0 + 128, :], writes=[wi[b2]])
                    nkeys = (qi + 1) * 128
                    nch = (nkeys + 511) // 512
                    for c in range(nch):
                        c0 = c * 512
                        cw = min(512, nkeys - c0)
                        for h in range(8):
                            pk = h % 2
                            pe([lambda e: e.matmul(k.pb(pk)[:, 0:cw], lhsT=qiT[b2].t[:, h, :], rhs=kiT.t[:, c0:c0 + cw], start=True, stop=True)], [qiT[b2], kiT], [P[pk]])
                            a_ = arel[ai % 3]
                            ai += 1
                            op("scalar", lambda e: e.activation(out=a_.t[:, 0:cw], in_=k.pb(pk)[:, 0:cw], func=AF.Relu), [P[pk]], [a_])
                            if h == 0:
                                op("vector", lambda e: e.tensor_scalar(out=I.t[:, c0:c0 + cw], in0=a_.t[:, 0:cw], scalar1=wi[b2].t[:, 0:1], scalar2=None, op0=ALU.mult), [a_, wi[b2]], [I])
                            else:
                                op("vector", lambda e: e.scalar_tensor_tensor(out=I.t[:, c0:c0 + cw], in0=a_.t[:, 0:cw], scalar=wi[b2].t[:, h:h + 1], in1=I.t[:, c0:c0 + cw], op0=ALU.mult, op1=ALU.add), [a_, wi[b2]], [I])
                        op("vector", lambda e: e.tensor_reduce(out=amp.t[:, c:c + 1], in_=I.t[:, c0:c0 + cw], axis=AX.X, op=ALU.max, apply_absolute_value=True), [I], [amp])
                    op("vector", lambda e: e.tensor_tensor(out=I.t[:, tok0:tok0 + 128], in0=I.t[:, tok0:tok0 + 128], in1=cneg.t[:], op=ALU.add), [cneg], [I])
                    op("vector", lambda e: e.tensor_reduce(out=amax.t[:], in_=amp.t[:, 0:nch], axis=AX.X, op=ALU.max), [amp], [amax])
                    op("vector", lambda e: e.tensor_scalar(out=lo.t[:], in0=amax.t[:], scalar1=-1.0, scalar2=None, op0=ALU.mult), [amax], [lo])
                    op("vector", lambda e: e.tensor_scalar(out=wall.t[:], in0=pow2.t[:], scalar1=amax.t[:, 0:1], scalar2=None, op0=ALU.mult), [pow2, amax], [wall])
                    if nkeys > 256:
                        for it in range(NBIS):
                            op("vector", lambda e: e.tensor_tensor(out=mid.t[:], in0=lo.t[:], in1=wall.t[:, it:it + 1], op=ALU.add), [lo, wall], [mid])
                            for a0 in range(0, nkeys, 2048):
                                a1 = min(nkeys, a0 + 2048)
                                first = (a0 == 0)
                                op("vector", lambda e: e.tensor_scalar(out=junk.t[:, 0:a1 - a0], in0=I.t[:, a0:a1], scalar1=mid.t[:, 0:1], scalar2=(None if first else cnt.t[:, 0:1]),
                                                                       op0=ALU.is_ge, op1=ALU.add, accum_out=cnt.t[:, 0:1]), [I, mid] + ([] if first else [cnt]), [junk, cnt])
                            op("vector", lambda e: e.scalar_tensor_tensor(out=tmp1.t[:], in0=cnt.t[:], scalar=255.5, in1=wall.t[:, it:it + 1], op0=ALU.is_ge, op1=ALU.mult), [cnt, wall], [tmp1])
                            op("vector", lambda e: e.tensor_tensor(out=lo.t[:], in0=lo.t[:], in1=tmp1.t[:], op=ALU.add), [tmp1], [lo])
                    nm = negm[b2]
                    op("vector", lambda e: e.tensor_scalar(out=nm.t[:, 0:nkeys], in0=I.t[:, 0:nkeys], scalar1=lo.t[:, 0:1], scalar2=NEG, op0=ALU.is_lt, op1=ALU.mult), [I, lo], [nm])
                    aic[0] = ai

                def dsa_phase(qi):
                    tok0 = qi * 128
                    b2 = qi % 2
                    nm = negm[b2]
                    pend = [None]
                    for j in range(qi + 1):
                        d = qi - j
                        near = d <= 5
                        fns = []
                        for half in range(2):
                            pk = 2 + half
                            fns.append(lambda e, pk=pk, half=half: e.matmul(k.pb(pk), lhsT=ckvT.t[:, j * 128:(j + 1) * 128], rhs=qlT[b2].t[:, half * 4:(half + 1) * 4, :], start=True, stop=False))
                            fns.append(lambda e, pk=pk, half=half: e.matmul(k.pb(pk), lhsT=nm.t[:, j * 128:(j + 1) * 128], rhs=sel8.t[:, 0:4, :], start=False, stop=(not near)))
                            if near:
                                fns.append(lambda e, pk=pk, half=half: e.matmul(k.pb(pk), lhsT=identb.t[:], rhs=biasA.t[:, d, half * 4:(half + 1) * 4, :], start=False, stop=True))
                        pe(fns, [ckvT, qlT[b2], nm, sel8, identb, biasA], [P[2], P[3]])
                        pt = PT[j % 2]
                        op("scalar", lambda e: e.activation(out=pt.t[:], in_=k.pb2(1), func=AF.Exp, scale=scale), [P[2], P[3]], [pt])
                        cur = (j, pt)
                        for jj, pt_ in ([pend[0]] if pend[0] is not None else []) + ([cur] if j == qi else []):
                            fns = []
                            for half in range(2):
                                fns.append(lambda e, half=half: e.matmul(k.pb(4 + half), lhsT=ckv.t[:, jj, :], rhs=pt_.t[:, half * 512:(half + 1) * 512], start=(jj == 0), stop=(jj == qi)))
                                fns.append(lambda e, half=half: e.matmul(k.pb(6 + half), lhsT=onesb.t[:], rhs=pt_.t[:, half * 512:(half + 1) * 512], start=(jj == 0), stop=(jj == qi)))
                            pe(fns, [ckv, pt_, onesb], [P[4], P[5], P[6], P[7]])
                        pend[0] = cur if j < qi else None
                    op("vector", lambda e: e.reciprocal(out=rz.t[:], in_=k.pb2(3)), [P[6], P[7]], [rz])
                    op("vector", lambda e: e.tensor_tensor(out=oaT.t[:].rearrange("p a b -> p (a b)"), in0=k.pb2(2), in1=rz.t[:], op=ALU.mult), [P[4], P[5], rz], [oaT])
                    fns = []
                    for h in range(8):
                        fns.append(lambda e, h=h: e.matmul(k.pb(0)[(h % 2) * 64:(h % 2) * 64 + 64, (h // 2) * 128:(h // 2 + 1) * 128], lhsT=wuv.t[:, h, :], rhs=oaT.t[:, h, :], start=True, stop=True))
                    pe(fns, [wuv, oaT], [P[0]])
                    ob = oub[b2]
                    op("vector", lambda e: e.tensor_copy(out=ob.t[:].rearrange("p a b -> p (a b)"), in_=k.pb(0)), [P[0]], [ob])
                    dma("gpsimd", OT8[0:4, :, tok0:tok0 + 128].rearrange("b p t -> p b t"), ob.t[:], reads=[ob])

                idx_phase(0)
                for qi in range(NTR):
                    if qi + 1 < NTR:
                        idx_phase(qi + 1)
                    dsa_phase(qi)

        def attn_diff(l):
            e_ = l // 2
            scale = 64 ** -0.5
            with Stage(k) as st:
                kbT = st.sb("kbT", [64, 2, T], BF16)
                vbh = st.sb("vbh", [128, 64, 128], BF16)
                dbias = st.sb("dbias", [128, 9, 4, 128], BF16)
                qbT = [st.sb(f"qbT{i}", [64, 512], BF16) for i in range(2)]
                PT = [st.sb(f"PT{i}", [128, 512], BF16) for i in range(3)]
                rzz = [st.sb(f"rzz{i}", [128, 512], F32) for i in range(2)]
                aa = [st.sb(f"aa{i}", [128, 512], F32) for i in range(2)]
                oo = st.sb("oo", [128, 512], F32)
                sq = st.sb("sq", [128, 512], F32)
                rs_ = st.sb("rs_", [128, 512], F32)
                obT = [st.sb(f"obT{i}", [128, 512], BF16) for i in range(2)]
                pti = 0
                for h in range(4):
                    for c0 in range(0, NTR * 128, 2048):
                        c1 = min(NTR * 128, c0 + 2048)
                        for m in range(2):
                            dma("sync", kbT.t[:, m, c0:c1], KBT[2 * h + m, :, c0:c1], writes=[kbT])
                        dma("sync", vbh.t[:, c0 // 128:c1 // 128, :], VB[c0:c1, h * 128:(h + 1) * 128].rearrange("(j p) e -> p j e", p=128), writes=[vbh])
                    for ri in range(9):
                        r = ri - 5
                        for t_ in range(4):
                            d = t_ - r
                            if d < 0:
                                op("gpsimd", lambda e: e.memset(dbias.t[:, ri, t_, :], NEG), [], [dbias])
                            elif d <= 5:
                                dma("sync", dbias.t[:, ri, t_, :], Bscr[8 + h, d, :, :], writes=[dbias])
                            else:
                                op("gpsimd", lambda e: e.memset(dbias.t[:, ri, t_, :], 0.0), [], [dbias])
                    for g in range(NGR):
                        tok0 = g * 512
                        nblk = 4 * g + 4
                        for m in range(2):
                            qb = qbT[m]
                            dma("sync", qb.t[:], QBT[2 * h + m, :, tok0:tok0 + 512], writes=[qb])
                            pend = None
                            for j in range(nblk):
                                r = j - 4 * g
                                pk = j % 2
                                fns = [lambda e: e.matmul(k.pb(pk), lhsT=kbT.t[:, m, j * 128:(j + 1) * 128], rhs=qb.t[:], start=True, stop=(r < -5))]
                                if r >= -5:
                                    fns.append(lambda e: e.matmul(k.pb(pk), lhsT=identb.t[:], rhs=dbias.t[:, r + 5, :, :], start=False, stop=True))
                                pe(fns, [kbT, qb, identb, dbias], [P[pk]])
                                pt = PT[pti % 3]
                                pti += 1
                                op("scalar", lambda e: e.activation(out=pt.t[:], in_=k.pb(pk), func=AF.Exp, scale=scale), [P[pk]], [pt])
                                cur = (j, pt)
                                for jj, pt_ in ([pend] if pend is not None else []) + ([cur] if j == nblk - 1 else []):
                                    pe([lambda e: e.matmul(k.pb(2 + m), lhsT=vbh.t[:, jj, :], rhs=pt_.t[:], start=(jj == 0), stop=(jj == nblk - 1)),
                                        lambda e: e.matmul(k.pb(4 + m), lhsT=onesb.t[:], rhs=pt_.t[:], start=(jj == 0), stop=(jj == nblk - 1))], [vbh, pt_, onesb], [P[2 + m], P[4 + m]])
                                pend = cur
                            op("vector", lambda e: e.reciprocal(out=rzz[m].t[:], in_=k.pb(4 + m)), [P[4 + m]], [rzz[m]])
                            op("vector", lambda e: e.tensor_tensor(out=aa[m].t[:], in0=k.pb(2 + m), in1=rzz[m].t[:], op=ALU.mult), [P[2 + m], rzz[m]], [aa[m]])
                        op("vector", lambda e: e.scalar_tensor_tensor(out=oo.t[:], in0=aa[1].t[:], scalar=neglam.t[:, e_:e_ + 1], in1=aa[0].t[:], op0=ALU.mult, op1=ALU.add), [aa[0], aa[1], neglam], [oo])
                        op("scalar", lambda e: e.activation(out=sq.t[:], in_=oo.t[:], func=AF.Square), [oo], [sq])
                        pe([lambda e: e.matmul(k.pb(6), lhsT=onesf.t[:], rhs=sq.t[:], start=True, stop=True)], [onesf, sq], [P[6]])
                        op("scalar", lambda e: e.activation(out=rs_.t[:], in_=k.pb(6), func=AF.Sqrt, bias=epsb.t[:, 0:1], scale=1.0 / 128), [P[6], epsb], [rs_])
                        op("vector", lambda e: e.reciprocal(out=rs_.t[:], in_=rs_.t[:]), [rs_], [rs_])
                        ob = obT[g % 2]
                        op("vector", lambda e: e.scalar_tensor_tensor(out=ob.t[:], in0=oo.t[:], scalar=gsub.t[:, e_:e_ + 1], in1=rs_.t[:], op0=ALU.mult, op1=ALU.mult), [oo, gsub, rs_], [ob])
                        dma("gpsimd", OT8[4 + h, :, tok0:tok0 + 512], ob.t[:], reads=[ob])

        def proj_odd(l):
            o_ = l // 2
            with Stage(k) as st:
                win = st.sb("win", [128, 8, OD_COLS], BF16)
                for kc in range(8):
                    dma("sync", win.t[:, kc, :], od_w_in_b[o_][kc * 128:(kc + 1) * 128, :], writes=[win])
                g0 = st.sb("g0", [128, D], F32)
                dma("sync", g0.t[:], norm_g[l, 0:1, :].partition_broadcast(128), writes=[g0])
                xt = [st.sb(f"xt{i}", [128, D], F32) for i in range(4)]
                hb = st.sb("hb", [128, D], BF16)
                junk = st.sb("junk", [128, D], BF16)
                hT = st.sb("hT", [128, 8, 512], BF16)
                ss = st.sb("ss", [128, 4], F32)
                rstd = st.sb("rstd", [128, 4], F32)
                fm = [st.sb(f"fm{i}", [128, 512], BF16) for i in range(3)]
                v_sb = [st.sb(f"v_sb{i}", [128, 128], BF16) for i in range(2)]
                fmi = 0
                for g in range(NGR):
                    tok0 = g * 512
                    for t_ in range(4):
                        dma("sync", xt[t_].t[:], out[tok0 + t_ * 128:tok0 + (t_ + 1) * 128, :], writes=[xt[t_]])
                    norm_to_hT(st, xt, g0, hb, hT, junk, ss, rstd, [0, 1])
                    blocks = []
                    for p_ in range(8):
                        blocks.append((p_ * 128, QT[2 * p_:2 * p_ + 2].rearrange("a d t -> (a d) t")))
                    blocks.append((1024, KT.rearrange("a d t -> (a d) t")))
                    for bi, (c0, dest) in enumerate(blocks):
                        pk = 2 + bi % 2
                        pe([(lambda e, kc=kc: e.matmul(k.pb(pk), lhsT=win.t[:, kc, c0:c0 + 128], rhs=hT.t[:, kc, :], start=(kc == 0), stop=(kc == 7))) for kc in range(8)], [win, hT], [P[pk]])
                        f_ = fm[fmi % 3]
                        fmi += 1
                        k.copy(k.evac_eng(), f_.t[:], k.pb(pk), [P[pk]], [f_])
                        dma("gpsimd", dest[:, tok0:tok0 + 512], f_.t[:], reads=[f_])
                    for t_ in range(4):
                        r0 = tok0 + t_ * 128
                        pe([(lambda e, kc=kc: e.matmul(k.pb(4)[:, 0:128], lhsT=hT.t[:, kc, t_ * 128:(t_ + 1) * 128], rhs=win.t[:, kc, 1152:1280], start=(kc == 0), stop=(kc == 7))) for kc in range(8)], [win, hT], [P[4]])
                        v_ = v_sb[t_ % 2]
                        k.copy(k.evac_eng(), v_.t[:], k.pb(4)[:, 0:128], [P[4]], [v_])
                        dma("gpsimd", VV[r0:r0 + 128, :], v_.t[:], reads=[v_])

        def attn_swa(l):
            o_ = l // 2
            scale = 64 ** -0.5
            with Stage(k) as st:
                kT = st.sb("kT", [64, 2, T], BF16)
                vall = st.sb("vall", [128, 64, 128], BF16)
                for c0 in range(0, NTR * 128, 2048):
                    c1 = min(NTR * 128, c0 + 2048)
                    for kv in range(2):
                        dma("sync", kT.t[:, kv, c0:c1], KT[kv, :, c0:c1], writes=[kT])
                    dma("sync", vall.t[:, c0 // 128:c1 // 128, :], VV[c0:c1, :].rearrange("(j p) e -> p j e", p=128), writes=[vall])
                biasC = st.sb("biasC", [128, 2, 16, 128], BF16)
                for d in range(2):
                    dma("sync", biasC.t[:, d, :, :], Bscr[12:28, d, :, :].rearrange("h s q -> s h q"), writes=[biasC])
                op("vector", lambda e: e.memset(biasC.t[0:64, 1, :, 64:128], NEG), [], [biasC])
                sk = st.sb("sk", [1, 16], F32)
                es16 = st.sb("es16", [1, 16], F32)
                esrow = st.sb("esrow", [1, 16, 128], F32)
                dma("sync", sk.t[:], od_sinks[o_:o_ + 1, :], writes=[sk])
                op("scalar", lambda e: e.activation(out=es16.t[:], in_=sk.t[:], func=AF.Exp), [sk], [es16])
                for hh in range(16):
                    op("vector", lambda e: e.tensor_scalar(out=esrow.t[0:1, hh, :], in0=onesf.t[0:1, :], scalar1=es16.t[0:1, hh:hh + 1], scalar2=None, op0=ALU.mult), [onesf, es16], [esrow])
                qT = [st.sb(f"qT{i}", [64, 8, 128], BF16) for i in range(2)]
                PT = [st.sb(f"PT{i}", [128, 1024], BF16) for i in range(2)]
                rz = st.sb("rz", [64, 1024], F32)
                osb = [st.sb(f"osb{i}", [64, 8, 128], BF16) for i in range(2)]
                it = 0
                for qi in range(NTR):
                    tok0 = qi * 128
                    for kv in range(2):
                        q_ = qT[it % 2]
                        dma("sync", q_.t[:], QT[kv * 8:(kv + 1) * 8, :, tok0:tok0 + 128].rearrange("g d t -> d g t"), writes=[q_])
                        blks = ([(qi - 1, 1)] if qi >= 1 else []) + [(qi, 0)]
                        for bi, (j, d) in enumerate(blks):
                            fns = []
                            for half in range(2):
                                pk = 2 * (bi % 2) + half
                                fns.append(lambda e, pk=pk, half=half: e.matmul(k.pb(pk), lhsT=kT.t[:, kv, j * 128:(j + 1) * 128], rhs=q_.t[:, half * 4:(half + 1) * 4, :], start=True, stop=False))
                                fns.append(lambda e, pk=pk, half=half: e.matmul(k.pb(pk), lhsT=identb.t[:], rhs=biasC.t[:, d, kv * 8 + half * 4:kv * 8 + half * 4 + 4, :], start=False, stop=True))
                            pp_i = bi % 2
                            pe(fns, [kT, q_, identb, biasC], [P[2 * pp_i], P[2 * pp_i + 1]])
                            pt = PT[bi % 2]
                            op("scalar", lambda e: e.activation(out=pt.t[:], in_=k.pb2(pp_i), func=AF.Exp, scale=scale), [P[2 * pp_i], P[2 * pp_i + 1]], [pt])
                            fns = []
                            last = (bi == len(blks) - 1)
                            for half in range(2):
                                fns.append(lambda e, half=half: e.matmul(k.pb(4 + half)[0:64, :], lhsT=vall.t[:, j, kv * 64:(kv + 1) * 64], rhs=pt.t[:, half * 512:(half + 1) * 512], start=(bi == 0), stop=last))
                                fns.append(lambda e, half=half: e.matmul(k.pb(6 + half)[0:64, :], lhsT=onesb.t[:, 0:64], rhs=pt.t[:, half * 512:(half + 1) * 512], start=(bi == 0), stop=False))
                                if last:
                                    fns.append(lambda e, half=half: e.matmul(k.pb(6 + half)[0:64, :], lhsT=onesf.t[0:1, 0:64], rhs=esrow.t[0:1, kv * 8 + half * 4:kv * 8 + half * 4 + 4, :], start=False, stop=True))
                            pe(fns, [vall, pt, onesb, onesf, esrow], [P[4], P[5], P[6], P[7]])
                        op("vector", lambda e: e.reciprocal(out=rz.t[:], in_=k.pb2(3)[0:64, :]), [P[6], P[7]], [rz])
                        o_sb = osb[it % 2]
                        op("vector", lambda e: e.tensor_tensor(out=o_sb.t[:].rearrange("p a b -> p (a b)"), in0=k.pb2(2)[0:64, :], in1=rz.t[:], op=ALU.mult), [P[4], P[5], rz], [o_sb])
                        dma("gpsimd", OT16[kv * 8:(kv + 1) * 8, :, tok0:tok0 + 128].rearrange("g d t -> d g t"), o_sb.t[:], reads=[o_sb])
                        it += 1

        def tail(l):
            even = (l % 2 == 0)
            KP, nblk = (128, 8) if even else (64, 16)
            wsrc = ev_w_out_b[l // 2] if even else od_w_out_b[l // 2]
            OT = OT8 if even else OT16
            xscale = 64 ** -0.5
            tsub = stop[5] if (stop is not None and stop.startswith(f"tail{l}") and len(stop) == 6) else None
            with Stage(k) as st:
                wout = st.sb("wout", [KP, nblk, D], BF16)
                for c in range(4):
                    dma("sync", wout.t[:, c * (nblk // 4):(c + 1) * (nblk // 4), :], wsrc[c * 256:(c + 1) * 256, :].rearrange("(b p) n -> p b n", p=KP), writes=[wout])
                wq = st.sb("wq", [128, 8, 256], BF16)
                dma("sync", wq.t[:], wq_b[l].rearrange("(kc p) n -> p kc n", p=128), writes=[wq])
                wo = st.sb("wo", [64, 4, D], BF16)
                dma("sync", wo.t[:], wo_b[l].rearrange("(h d) n -> d h n", d=64), writes=[wo])
                kx = st.sb("kx", [64, 4, 256], BF16)
                vx = st.sb("vx", [128, 2, 256], BF16)
                dma("sync", kx.t[:], KXT[l], writes=[kx])
                dma("sync", vx.t[:], VX[l].rearrange("(i p) n -> p i n", p=128), writes=[vx])
                gb = []
                for i in range(1, 6):
                    g_ = st.sb(f"g{i}", [128, D], F32)
                    dma("sync", g_.t[:], norm_g[l, i:i + 1, :].partition_broadcast(128), writes=[g_])
                    gb.append(g_)
                g1, g2, g3, g4, g5 = gb
                xt = [st.sb(f"xt{i}", [128, D], F32) for i in range(4)]
                ysb = [st.sb(f"ysb{i}", [128, D], F32) for i in range(4)]
                tmp = st.sb("tmp", [128, D], F32)
                hb = st.sb("hb", [128, D], BF16)
                junk = st.sb("junk", [128, D], BF16)
                hT = st.sb("hT", [128, 8, 512], BF16)
                oT = st.sb("oT", [KP, nblk, 512], BF16)
                uT = st.sb("uT", [128, 32, 512], BF16)
                rr_ = st.sb("rr_", [128, 512], BF16)
                ws = [st.sb(f"ws{i}", [128, 8, 512], BF16) for i in range(3)]
                ss = st.sb("ss", [128, 4], F32)
                rstd = st.sb("rstd", [128, 4], F32)
                sspart = st.sb("sspart", [128, 8], F32)
                qx = st.sb("qx", [64, 4, 512], BF16)
                PTx = [st.sb(f"PTx{i}", [128, 512], BF16) for i in range(2)]
                rzx = st.sb("rzx", [64, 512], F32)
                ox = st.sb("ox", [64, 4, 512], BF16)
                xsrc = x_in if l == 0 else out
                wsi = 0
                for g in range(NGR if tsub != "s" else 0):
                    tok0 = g * 512
                    for t_ in range(4):
                        dma("sync", xt[t_].t[:], xsrc[tok0 + t_ * 128:tok0 + (t_ + 1) * 128, :], writes=[xt[t_]])
                    dma("sync", oT.t[:], OT[:, :, tok0:tok0 + 512].rearrange("b p t -> p b t"), writes=[oT])
                    if tsub == "x":
                        for t_ in range(4):
                            dma("gpsimd", out[tok0 + t_ * 128:tok0 + (t_ + 1) * 128, :], xt[t_].t[:], reads=[xt[t_]])
                        continue
                    for half in range(2):
                        for t_ in range(4):
                            pk = t_
                            pe([(lambda e, b=b: e.matmul(k.pb(pk), lhsT=oT.t[:, b, t_ * 128:(t_ + 1) * 128], rhs=wout.t[:, b, half * 512:(half + 1) * 512], start=(b == 0), stop=(b == nblk - 1))) for b in range(nblk)], [oT, wout], [P[pk]])
                            evac_y(pk, ysb[t_], half, sspart, t_, junk)
                    if tsub == "m":
                        for t_ in range(4):
                            dma("gpsimd", out[tok0 + t_ * 128:tok0 + (t_ + 1) * 128, :], ysb[t_].t[:], reads=[ysb[t_]])
                        continue
                    post_norm_add(st, xt, ysb, sspart, g1, rstd, tmp)
                    if tsub == "a":
                        for t_ in range(4):
                            dma("gpsimd", out[tok0 + t_ * 128:tok0 + (t_ + 1) * 128, :], xt[t_].t[:], reads=[xt[t_]])
                        continue
                    norm_to_hT(st, xt, g2, hb, hT, junk, ss, rstd, [4, 5])
                    for h in range(4):
                        pk = 6 + h % 2
                        pe([(lambda e, kc=kc: e.matmul(k.pb(pk)[0:64, :], lhsT=wq.t[:, kc, h * 64:(h + 1) * 64], rhs=hT.t[:, kc, :], start=(kc == 0), stop=(kc == 7))) for kc in range(8)], [wq, hT], [P[pk]])
                        k.copy(k.evac_eng(), qx.t[:, h, :], k.pb(pk)[0:64, :], [P[pk]], [qx])
                    for h in range(4):
                        for mb_ in range(2):
                            pk = mb_
                            pe([lambda e: e.matmul(k.pb(pk), lhsT=kx.t[:, h, mb_ * 128:(mb_ + 1) * 128], rhs=qx.t[:, h, :], start=True, stop=True)], [kx, qx], [P[pk]])
                            pt = PTx[mb_]
                            op("scalar", lambda e: e.activation(out=pt.t[:], in_=k.pb(pk), func=AF.Exp, scale=xscale), [P[pk]], [pt])
                            pe([lambda e: e.matmul(k.pb(2)[0:64, :], lhsT=vx.t[:, mb_, h * 64:(h + 1) * 64], rhs=pt.t[:], start=(mb_ == 0), stop=(mb_ == 1)),
                                lambda e: e.matmul(k.pb(3)[0:64, :], lhsT=onesb.t[:, 0:64], rhs=pt.t[:], start=(mb_ == 0), stop=(mb_ == 1))], [vx, pt, onesb], [P[2], P[3]])
                        op("vector", lambda e: e.reciprocal(out=rzx.t[:], in_=k.pb(3)[0:64, :]), [P[3]], [rzx])
                        op("vector", lambda e: e.tensor_tensor(out=ox.t[:, h, :], in0=k.pb(2)[0:64, :], in1=rzx.t[:], op=ALU.mult), [P[2], rzx], [ox])
                    for half in range(2):
                        for t_ in range(4):
                            pk = 4 + t_
                            pe([(lambda e, h=h: e.matmul(k.pb(pk), lhsT=ox.t[:, h, t_ * 128:(t_ + 1) * 128], rhs=wo.t[:, h, half * 512:(half + 1) * 512], start=(h == 0), stop=(h == 3))) for h in range(4)], [ox, wo], [P[pk]])
                            evac_y(pk, ysb[t_], half, sspart, t_, junk)
                    post_norm_add(st, xt, ysb, sspart, g3, rstd, tmp)
                    if tsub == "b":
                        for t_ in range(4):
                            dma("gpsimd", out[tok0 + t_ * 128:tok0 + (t_ + 1) * 128, :], xt[t_].t[:], reads=[xt[t_]])
                        continue
                    norm_to_hT(st, xt, g4, hb, hT, junk, ss, rstd, [0, 1])
                    for fb in range(8):
                        w_ = ws[wsi % 3]
                        wsi += 1
                        dma("sync", w_.t[:], w1_b[l][:, fb * 512:(fb + 1) * 512].rearrange("(kc p) n -> p kc n", p=128), writes=[w_])
                        for sub in range(4):
                            pk = 2 + sub % 2
                            pe([(lambda e, kc=kc: e.matmul(k.pb(pk), lhsT=w_.t[:, kc, sub * 128:(sub + 1) * 128], rhs=hT.t[:, kc, :], start=(kc == 0), stop=(kc == 7))) for kc in range(8)], [w_, hT], [P[pk]])
                            op("scalar", lambda e: e.activation(out=rr_.t[:], in_=k.pb(pk), func=AF.Relu), [P[pk]], [rr_])
                            op("vector", lambda e: e.tensor_tensor(out=uT.t[:, fb * 4 + sub, :], in0=rr_.t[:], in1=rr_.t[:], op=ALU.mult), [rr_], [uT])
                    for half in range(2):
                        for fq in range(4):
                            w_ = ws[wsi % 3]
                            wsi += 1
                            dma("sync", w_.t[:], w2_b[l][fq * 1024:(fq + 1) * 1024, half * 512:(half + 1) * 512].rearrange("(fc p) n -> p fc n", p=128), writes=[w_])
                            for t_ in range(4):
                                pk = 4 + t_
                                pe([(lambda e, fc=fc: e.matmul(k.pb(pk), lhsT=uT.t[:, fq * 8 + fc, t_ * 128:(t_ + 1) * 128], rhs=w_.t[:, fc, :], start=(fq == 0 and fc == 0), stop=(fq == 3 and fc == 7))) for fc in range(8)], [uT, w_], [P[pk]])
                        for t_ in range(4):
                            evac_y(4 + t_, ysb[t_], half, sspart, t_, junk)
                    post_norm_add(st, xt, ysb, sspart, g5, rstd, tmp)
                    for t_ in range(4):
                        dma("gpsimd", out[tok0 + t_ * 128:tok0 + (t_ + 1) * 128, :], xt[t_].t[:], reads=[xt[t_]])

        seq = []
        for l in range(DEPTH):
            if l % 2 == 0:
                seq += [(f"proj{l}", proj_even), (f"dsa{l}", attn_dsa), (f"diff{l}", attn_diff), (f"tail{l}", tail)]
            else:
                seq += [(f"proj{l}", proj_odd), (f"swa{l}", attn_swa), (f"tail{l}", tail)]
        if stop != "pre":
            for name, fn in seq:
                fn(int(name[-1]))
                if done(name) or (stop is not None and stop[:5] == name and name.startswith("tail")):
                    break
        k.barrier()
    return nc


_NAMES = ["rel_bias_table", "norm_g", "ev_w_in", "ev_a_kv_norm", "ev_a_w_uk", "ev_a_w_uv", "ev_b_lambda", "ev_b_subln",
          "ev_w_out", "od_w_in", "od_sinks", "od_w_out", "xa_wq", "xa_wkv", "xa_wo", "xa_mem_norm", "mlp_w1", "mlp_w2"]


def make_in_maps(inputs):
    oh, c128, hmask = host_consts()
    shared = {n: np.ascontiguousarray(np.asarray(inputs[n], dtype=np.float32)) for n in _NAMES}
    x = np.asarray(inputs["x"], dtype=np.float32)
    mem = np.asarray(inputs["mem"], dtype=np.float32)
    in_maps = []
    for c in range(8):
        b = c % 4
        m = dict(shared)
        m["x"] = np.ascontiguousarray(x[b])
        m["mem"] = np.ascontiguousarray(mem[b])
        m["c_oh"] = oh
        m["c_128"] = c128
        m["c_hmask"] = hmask
        in_maps.append(m)
    return in_maps


def kernel(**inputs):
    nc = build()
    in_maps = make_in_maps(inputs)
    res = run_bass_kernel_spmd(nc, in_maps[:4], core_ids=list(range(4)))
    return np.stack([np.asarray(res.results[b]["out"], dtype=np.float32) for b in range(4)], axis=0)
```
